# Optimizing a Trainium2 kernel written in Bass

```python
import jax, jax.numpy as jnp
from jax import lax
import numpy as np

D_MODEL = 2048
BATCH = 8
SEQ = 2048
DEPTH = 1

MEM_LEN = 256
EPS = 1e-6

GLA_HEADS = 4
GLA_DK = D_MODEL // (4 * GLA_HEADS)
GLA_DV = D_MODEL // (2 * GLA_HEADS)
GLA_GATE_RANK = 16
GLA_TAU = 16.0
GLA_CHUNK = 64

SG_GROUPS = 4
SG_DIM = D_MODEL // (2 * SG_GROUPS)
SG_CHUNK = 128

XA_HEADS = 4
XA_DIM = D_MODEL // (2 * XA_HEADS)

BRANCH_W = D_MODEL // 2
N_BRANCH = 3
D_FF = 256 * ((8 * D_MODEL // 3 + 255) // 256)

IN_SPLITS = (GLA_HEADS * GLA_DK,
             GLA_HEADS * GLA_DK,
             GLA_HEADS * GLA_DV,
             GLA_HEADS * GLA_DV,
             GLA_GATE_RANK,
             SG_GROUPS * SG_DIM,
             SG_GROUPS * SG_DIM,
             XA_HEADS * XA_DIM,
             N_BRANCH * D_MODEL)
IN_WIDTH = 12304

kernel_name = "hybrid_gla_gmlp_memxattn_macaron"


def rmsnorm(x, g):
    xf = x.astype(jnp.float32)
    y = xf * lax.rsqrt(jnp.mean(xf * xf, axis=-1, keepdims=True) + EPS)
    return (y * g.astype(jnp.float32)).astype(x.dtype)


def swiglu(h, w_gate, w_up, w_down):
    return (jax.nn.silu(h @ w_gate) * (h @ w_up)) @ w_down


def gla(q, k, v, log_a, r, out_norm):
    B, S, H, dk = q.shape
    dv = v.shape[-1]
    C = GLA_CHUNK
    n = S // C

    def to_chunks(t):
        return t.astype(jnp.float32).reshape(B, n, C, H, t.shape[-1]).transpose(1, 0, 3, 2, 4)

    qc = to_chunks(q) * (dk ** -0.5)
    kc, vc, gc = to_chunks(k), to_chunks(v), to_chunks(log_a)
    mask = jnp.tril(jnp.ones((C, C), dtype=bool))[:, :, None]

    def step(state, inp):
        qb, kb, vb, gb = inp
        b = jnp.cumsum(gb, axis=2)
        inter = jnp.einsum('bhtd,bhde->bhte', qb * jnp.exp(b), state)
        diff = b[:, :, :, None, :] - b[:, :, None, :, :]
        decay = jnp.exp(jnp.where(mask, diff, -jnp.inf))
        scores = jnp.einsum('bhtd,bhsd,bhtsd->bhts', qb, kb, decay)
        intra = jnp.einsum('bhts,bhse->bhte', scores, vb)
        b_last = b[:, :, -1:, :]
        new_state = (jnp.exp(b_last[:, :, 0, :])[..., None] * state
                     + jnp.einsum('bhsd,bhse->bhde', kb * jnp.exp(b_last - b), vb))
        return new_state, inter + intra

    s0 = jnp.zeros((B, H, dk, dv), jnp.float32)
    _, o = lax.scan(step, s0, (qc, kc, vc, gc))
    o = o.transpose(1, 0, 3, 2, 4).reshape(B, S, H, dv)
    o = o * lax.rsqrt(jnp.mean(o * o, axis=-1, keepdims=True) + EPS) * out_norm.astype(jnp.float32)
    o = o * jax.nn.silu(r.astype(jnp.float32))
    return o.reshape(B, S, H * dv).astype(v.dtype)


def spatial_gating(u_pre, v_pre, ln_g, ln_b, w_s, b_s):
    B, S, _ = u_pre.shape
    G, c, T = SG_GROUPS, SG_DIM, SG_CHUNK
    u = jax.nn.gelu(u_pre, approximate=False).reshape(B, S, G, c)
    v = jax.nn.gelu(v_pre, approximate=False).reshape(B, S, G, c).astype(jnp.float32)
    mu = jnp.mean(v, axis=-1, keepdims=True)
    var = jnp.mean((v - mu) ** 2, axis=-1, keepdims=True)
    v = (v - mu) * lax.rsqrt(var + EPS) * ln_g.astype(jnp.float32) + ln_b.astype(jnp.float32)
    v = v.reshape(B, S // T, T, G, c)
    w = w_s.astype(jnp.float32) * jnp.tril(jnp.ones((T, T), jnp.float32))
    v = jnp.einsum('gts,bnsgc->bntgc', w, v) + b_s.astype(jnp.float32).T[:, :, None]
    out = u.astype(jnp.float32) * v.reshape(B, S, G, c)
    return out.reshape(B, S, G * c).astype(u_pre.dtype)


def memory_attention(q, mem_n, w_kv_mem):
    B, S, H, dh = q.shape
    M = mem_n.shape[1]
    kv = mem_n @ w_kv_mem
    k, v = jnp.split(kv, 2, axis=-1)
    k = k.reshape(B, M, H, dh)
    v = v.reshape(B, M, H, dh)
    s = jnp.einsum('bshd,bmhd->bhsm', q, k).astype(jnp.float32) * (dh ** -0.5)
    p = jax.nn.softmax(s, axis=-1).astype(v.dtype)
    o = jnp.einsum('bhsm,bmhd->bshd', p, v)
    return o.reshape(B, S, H * dh)


def mixer(h, mem_n, w_in, gla_w_gate_up, gla_gate_bias, gla_out_norm,
          sg_ln_g, sg_ln_b, sg_w_s, sg_b_s, w_kv_mem, w_branch, w_out):
    B, S, D = h.shape
    offsets = []
    acc = 0
    for width in IN_SPLITS[:-1]:
        acc += width
        offsets.append(acc)
    proj = h @ w_in
    q, k, v, r, g_lr, su, sv, xq, gate_pre = jnp.split(proj, offsets, axis=-1)

    log_a = jax.nn.log_sigmoid((g_lr @ gla_w_gate_up + gla_gate_bias).astype(jnp.float32)) / GLA_TAU
    y_gla = gla(q.reshape(B, S, GLA_HEADS, GLA_DK), k.reshape(B, S, GLA_HEADS, GLA_DK),
                v.reshape(B, S, GLA_HEADS, GLA_DV), log_a.reshape(B, S, GLA_HEADS, GLA_DK),
                r.reshape(B, S, GLA_HEADS, GLA_DV), gla_out_norm)
    y_sg = spatial_gating(su, sv, sg_ln_g, sg_ln_b, sg_w_s, sg_b_s)
    y_xa = memory_attention(xq.reshape(B, S, XA_HEADS, XA_DIM), mem_n, w_kv_mem)

    gates = jax.nn.sigmoid(gate_pre.astype(jnp.float32)).reshape(B, S, N_BRANCH, D)
    merged = jnp.zeros((B, S, D), jnp.float32)
    for i, y in enumerate((y_gla, y_sg, y_xa)):
        merged = merged + gates[:, :, i, :] * (y @ w_branch[i]).astype(jnp.float32)
    return merged.astype(h.dtype) @ w_out


def setup_inputs(seed: int = 0) -> dict:
    key = jax.random.key(seed)
    ks = jax.random.split(key, 24)
    f32 = jnp.float32
    L, D = DEPTH, D_MODEL

    def nrm(k, shape, scale):
        return jax.random.normal(k, shape, f32) * scale

    def gain(k, shape):
        return 1.0 + 0.01 * jax.random.normal(k, shape, f32)

    return {
        "x": jax.random.normal(ks[0], (BATCH, SEQ, D), f32),
        "mem": jax.random.normal(ks[1], (BATCH, MEM_LEN, D), f32),
        "ffn1_norm": gain(ks[2], (L, D)),
        "ffn1_w_gate": nrm(ks[3], (L, D, D_FF), D ** -0.5),
        "ffn1_w_up": nrm(ks[4], (L, D, D_FF), D ** -0.5),
        "ffn1_w_down": nrm(ks[5], (L, D_FF, D), D_FF ** -0.5),
        "mix_norm": gain(ks[6], (L, D)),
        "mem_norm": gain(ks[7], (L, D)),
        "w_in": nrm(ks[8], (L, D, IN_WIDTH), D ** -0.5),
        "gla_w_gate_up": nrm(ks[9], (L, GLA_GATE_RANK, GLA_HEADS * GLA_DK), GLA_GATE_RANK ** -0.5),
        "gla_gate_bias": nrm(ks[10], (L, GLA_HEADS * GLA_DK), 0.01),
        "gla_out_norm": gain(ks[11], (L, GLA_HEADS, GLA_DV)),
        "sg_ln_g": gain(ks[12], (L, SG_GROUPS, SG_DIM)),
        "sg_ln_b": nrm(ks[13], (L, SG_GROUPS, SG_DIM), 0.01),
        "sg_w_s": nrm(ks[14], (L, SG_GROUPS, SG_CHUNK, SG_CHUNK), SG_CHUNK ** -0.5),
        "sg_b_s": gain(ks[15], (L, SG_GROUPS, SG_CHUNK)),
        "w_kv_mem": nrm(ks[16], (L, D, 2 * XA_HEADS * XA_DIM), D ** -0.5),
        "w_branch": nrm(ks[17], (L, N_BRANCH, BRANCH_W, D), BRANCH_W ** -0.5),
        "w_out": nrm(ks[18], (L, D, D), D ** -0.5),
        "ffn2_norm": gain(ks[19], (L, D)),
        "ffn2_w_gate": nrm(ks[20], (L, D, D_FF), D ** -0.5),
        "ffn2_w_up": nrm(ks[21], (L, D, D_FF), D ** -0.5),
        "ffn2_w_down": nrm(ks[22], (L, D_FF, D), D_FF ** -0.5),
        "final_norm": gain(ks[23], (D,)),
    }


def reference(x, mem, ffn1_norm, ffn1_w_gate, ffn1_w_up, ffn1_w_down, mix_norm, mem_norm,
              w_in, gla_w_gate_up, gla_gate_bias, gla_out_norm, sg_ln_g, sg_ln_b, sg_w_s, sg_b_s,
              w_kv_mem, w_branch, w_out, ffn2_norm, ffn2_w_gate, ffn2_w_up, ffn2_w_down, final_norm):
    for l in range(DEPTH):
        x = x + 0.5 * swiglu(rmsnorm(x, ffn1_norm[l]), ffn1_w_gate[l], ffn1_w_up[l], ffn1_w_down[l])
        h = rmsnorm(x, mix_norm[l])
        mem_n = rmsnorm(mem, mem_norm[l])
        x = x + mixer(h, mem_n, w_in[l], gla_w_gate_up[l], gla_gate_bias[l], gla_out_norm[l],
                      sg_ln_g[l], sg_ln_b[l], sg_w_s[l], sg_b_s[l], w_kv_mem[l], w_branch[l], w_out[l])
        x = x + 0.5 * swiglu(rmsnorm(x, ffn2_norm[l]), ffn2_w_gate[l], ffn2_w_up[l], ffn2_w_down[l])
    return rmsnorm(x, final_norm)
```

```python
import numpy as np
import concourse.bass as bass
import concourse.mybir as mybir
from concourse.bass_utils import run_bass_kernel_spmd

F32 = mybir.dt.float32
BF16 = mybir.dt.bfloat16
AF = mybir.ActivationFunctionType
ALU = mybir.AluOpType
AX = mybir.AxisListType

D = 2048
SEQ = 2048
DFF = 5632
EPS = 1e-6
C_Q, C_K, C_V, C_R, C_GLR, C_SU, C_SV, C_XQ, C_GATE = 0, 512, 1024, 2048, 3072, 3088, 4112, 5136, 6160


class Res:
    __slots__ = ("name", "lo", "hi", "w", "r", "al")

    def __init__(self, name, lo=None, hi=None):
        self.name, self.lo, self.hi = name, lo, hi
        self.w = None
        self.r = {}
        self.al = []


class Buf:
    __slots__ = ("ap", "res")

    def __init__(self, ap, res):
        self.ap, self.res = ap, res


class Sched:
    def __init__(self, nc):
        self.nc = nc
        self.eng = {"pe": nc.tensor, "act": nc.scalar, "dve": nc.vector, "pool": nc.gpsimd, "sp": nc.sync}
        self.sem = {}
        self.cnt = {}
        self.seen = {e: {} for e in self.eng}
        for e in self.eng:
            self.sem[e] = nc.alloc_semaphore("s_" + e)
            self.cnt[e] = 0
        self.all_res = []
        self.npe = 0
        self.phases = []

    def phase(self, name):
        self.phases.append((name, self.npe))

    def _sync(self, e, reads, writes):
        need = {}

        def add(ev):
            if ev is None:
                return
            k, v = ev
            if need.get(k, 0) < v:
                need[k] = v

        for r in reads:
            add(r.w)
            for t in r.al:
                add(t.w)
        for w in writes:
            for t in [w] + w.al:
                for k, v in t.r.items():
                    add((k, v))
                add(t.w)
        seen = self.seen[e]
        eng = self.eng[e]
        for k, v in need.items():
            if e == "pe" and k == "pe":
                continue
            if seen.get(k, 0) >= v:
                continue
            eng.wait_ge(self.sem[k], v)
            seen[k] = v

    def _post(self, key, val, reads, writes):
        for r in reads:
            r.r[key] = val
        for w in writes:
            w.w = (key, val)
            w.r = {}

    def op(self, e, reads, writes, fn):
        self._sync(e, reads, writes)
        ins = fn(self.eng[e])
        self.cnt[e] += 1
        ins.then_inc(self.sem[e], 1)
        self._post(e, self.cnt[e], reads, writes)

    def mm(self, out_ap, pairs, reads, writes, start=True, stop=True, signal=True):
        self._sync("pe", reads, writes)
        n = len(pairs)
        ins = None
        self.npe += n
        for i, (l, r) in enumerate(pairs):
            ins = self.nc.tensor.matmul(out_ap, lhsT=l, rhs=r, start=(start and i == 0), stop=(stop and i == n - 1))
        if signal:
            self.cnt["pe"] += 1
            ins.then_inc(self.sem["pe"], 1)
            self._post("pe", self.cnt["pe"], reads, writes)
        else:
            self._post("pe", self.cnt["pe"] + 1, reads, writes)

    def tr(self, out_ap, in_ap, ident_ap, reads, writes, signal=True):
        self._sync("pe", reads, writes)
        self.npe += 1
        ins = self.nc.tensor.transpose(out_ap, in_ap, ident_ap)
        if signal:
            self.cnt["pe"] += 1
            ins.then_inc(self.sem["pe"], 1)
            self._post("pe", self.cnt["pe"], reads, writes)
        else:
            self._post("pe", self.cnt["pe"] + 1, reads, writes)

    def dma(self, q, key, parts, reads, writes):
        if key not in self.sem:
            self.sem[key] = self.nc.alloc_semaphore("d_" + key)
            self.cnt[key] = 0
        self._sync(q, reads, writes)
        for (o, i) in parts:
            ins = self.eng[q].dma_start(out=o, in_=i)
            self.cnt[key] += 16
            ins.then_inc(self.sem[key], 16)
        self._post(key, self.cnt[key], reads, writes)


def build_nc():
    nc = bass.Bass("TRN2", target_bir_lowering=False)
    S = Sched(nc)

    def din(name, shape):
        return nc.dram_tensor(name, list(shape), F32, kind="ExternalInput").ap()

    x_d = din("x", [SEQ, D])
    mem_d = din("mem", [256, D])
    win_d = din("w_in", [D, 12304])
    ffw = []
    for i in (1, 2):
        ffw.append((din(f"f{i}g", [D, DFF]), din(f"f{i}u", [D, DFF]), din(f"f{i}d", [DFF, D])))
    wkv_d = din("w_kv", [D, D])
    wbr_d = din("w_br", [3072, D])
    wout_d = din("w_out", [D, D])
    gains_d = din("gains", [128, 80])
    gon_d = din("gon", [128, 8])
    lnbc_d = din("lnbc", [128, 2048])
    wst_d = din("wst", [128, 512])
    wgu_d = din("wgu", [17, 512])
    bs_d = din("bs", [1, 512])
    cst_d = din("cst", [128, 256])
    y_d = nc.dram_tensor("y", [SEQ, D], F32, kind="ExternalOutput").ap()

    TOTAL = 212480
    BIG = nc.alloc_sbuf_tensor("BIG", [128, TOTAL // 2], BF16)

    def carve(name, off, shape, dtype):
        esz = 2 if dtype == BF16 else 4
        n = 1
        for s_ in shape:
            n *= s_
        assert off % 4 == 0 and off + n * esz <= TOTAL, (name, off, n * esz)
        ap = BIG[:, off // 2: off // 2 + n * esz // 2]
        if dtype == F32:
            ap = ap.bitcast(F32)
        if len(shape) == 2:
            ap = ap.rearrange("p (a b) -> p a b", b=shape[1])
        res = Res(name, off, off + n * esz)
        for o in S.all_res:
            if o.lo < res.hi and res.lo < o.hi:
                o.al.append(res)
                res.al.append(o)
        S.all_res.append(res)
        return Buf(ap, res)

    X_OFF = 0
    HB_OFF = 65536
    W_OFF = 98304
    R_OFF = 131072
    E_OFF = 153600
    BS_OFF = 180224
    C_OFF = 182272
    MKT_OFF = 196608
    MV_OFF = 200704
    ST_OFF = 204800
    STB_OFF = 208896
    SM_OFF = 210944
    S1 = R_OFF
    S2 = HB_OFF + 16384

    X = carve("X", X_OFF, [16, 1024], F32)
    HB = carve("HB", HB_OFF, [16, 1024], BF16)
    HBS = carve("HBS", HB_OFF, [16, 512], BF16)
    WS = [carve(f"W{i}", W_OFF + i * 8192, [4096], BF16) for i in range(4)]
    H1 = carve("H1", R_OFF, [11, 1024], BF16)
    STG = [carve(f"STG{i}", R_OFF + i * 8192, [2048], F32) for i in range(2)]
    SGT = [carve(f"SGT{i}", E_OFF + i * 1024, [512], BF16) for i in range(4)]
    o = C_OFF
    GAINS = carve("GAINS", o, [80], F32); o += 320
    GON = carve("GON", o, [8], F32); o += 32
    IDENT = carve("IDENT", o, [128], F32); o += 512
    MASKU = carve("MASKU", o, [128], F32); o += 512
    ONESB = carve("ONESB", o, [128], BF16); o += 256
    ONESF = carve("ONESF", o, [128], F32); o += 512
    WGU = carve("WGU", o, [512], F32); o += 2048
    WST = carve("WST", o, [4, 128], BF16); o += 1024
    WGLR = carve("WGLR", o, [16, 16], BF16); o += 512
    LNBC = carve("LNBC", o, [2048], F32); o += 8192
    assert o <= C_OFF + 14336
    BSROW = carve("BSROW", BS_OFF, [512], F32)
    MKT = carve("MKT", MKT_OFF, [8, 256], BF16)
    MV = carve("MV", MV_OFF, [2, 1024], BF16)
    ST = [carve(f"ST{h}", ST_OFF + h * 1024, [256], F32) for h in range(4)]
    STB = [carve(f"STB{h}", STB_OFF + h * 512, [256], BF16) for h in range(4)]
    o = SM_OFF
    EBL = carve("EBL", o, [16], F32); o += 64
    BNS = carve("BNS", o, [2, 6], F32); o += 48
    BNA = carve("BNA", o, [2, 2], F32); o += 16
    LNR = carve("LNR", o, [2], F32); o += 8
    o += 8
    MXB = carve("MXB", o, [4], F32); o += 16
    NMX = carve("NMX", o, [4], F32); o += 16
    RSUM = carve("RSUM", o, [4], F32); o += 16
    RINV = carve("RINV", o, [4], F32); o += 16
    assert o <= TOTAL
    QT = carve("QT", S1 + 0, [4, 512], BF16)
    KT = carve("KT", S1 + 4096, [4, 512], BF16)
    KTM = carve("KTM", S1 + 8192, [4, 512], BF16)
    VTM = carve("VTM", S1 + 12288, [4, 1024], BF16)
    GLR = carve("GLR", S1 + 20480, [512], F32)
    LA = carve("LA", S2 + 0, [4, 512], F32)
    EBT = [carve(f"EBT{i}", S2 + 8192 + i * 2048, [512], F32) for i in range(2)]
    ENBT = [carve(f"ENBT{i}", S2 + 12288 + i * 2048, [512], F32) for i in range(2)]
    YGLA = carve("YGLA", E_OFF, [8, 512], BF16)
    YSG = carve("YSG", E_OFF + 8192, [8, 512], BF16)
    YXA = carve("YXA", E_OFF + 16384, [8, 512], BF16)
    ENBTM = carve("ENBTM", E_OFF + 24576, [512], F32)
    PTs = [carve(f"PT{i}", S2 + i * 256, [128], BF16) for i in range(2)]
    OSQ = [carve(f"OSQ{i}", S2 + 512 + i * 512, [256], BF16) for i in range(2)]
    RSG = [carve(f"RSG{i}", S2 + 1536 + i * 512, [128], F32) for i in range(2)]
    OT = [carve(f"OT{i}", S2 + 2560 + i * 1024, [2, 128], F32) for i in range(2)]
    GV = [carve(f"GV{i}", S2 + i * 2048, [512], F32) for i in range(2)]
    SVTM = carve("SVTM", S1 + 0, [4, 1024], BF16)
    XQT = carve("XQT", S1 + 8192, [8, 512], BF16)
    PF = [carve(f"PF{i}", S2 + 8192 + i * 4096, [4, 256], F32) for i in range(2)]
    PT2 = [carve(f"PT2{i}", S1 + 16384 + i * 2048, [8, 128], BF16) for i in range(2)]
    ACC = carve("ACC", S2 + 0, [4, 512], F32)
    GT = [carve(f"GT{i}", S2 + 8192 + i * 4096, [4, 512], BF16) for i in range(2)]
    MRG = [carve(f"MRG{i}", S1 + i * 4096, [4, 512], BF16) for i in range(2)]
    TT_ = [carve(f"TT{i}", S1 + 8192 + i * 2048, [512], F32) for i in range(2)]

    PS = []
    for i in range(8):
        t = nc.alloc_psum_tensor(f"ps{i}", [128, 512], F32)
        PS.append(Buf(t[:, :], Res(f"ps{i}")))
    state = {"pi": 0, "wi": 0}

    def psum():
        b = PS[state["pi"] % 8]
        state["pi"] += 1
        return b

    def wsrc(d2, r0, nk, c0, ncol):
        return d2[r0:r0 + nk * 128, c0:c0 + ncol].rearrange("(k p) c -> p k c", p=128)

    def wload(srcs):
        idx = state["wi"] % 4
        state["wi"] += 1
        slot = WS[idx]
        off = 0
        views, parts = [], []
        for s_ in srcs:
            nk, ncol = s_.shape[1], s_.shape[2]
            v = slot.ap[:, off:off + nk * ncol].rearrange("p (k c) -> p k c", c=ncol)
            parts.append((v, s_))
            views.append(v)
            off += nk * ncol
        assert off <= 4096
        S.dma("pool", f"w{idx}", parts, [], [slot.res])
        return slot.res, views

    def act(out, in_, func, reads, writes, **kw):
        S.op("act", reads, writes, lambda e: e.activation(out, in_, func, **kw))

    def rsqrt_from_psum(dst, ps_ap, scale, reads_ps, dres):
        act(dst, ps_ap, AF.Ln, [reads_ps], [dres], bias=EPS, scale=scale)
        act(dst, dst, AF.Exp, [dres], [dres], scale=-0.5)

    def rmsnorm(src, s_t0, dst, d_t0, gcol, ntok, tmp_off, blk=512):
        SQ = [carve(f"sq{tmp_off}_{i}", tmp_off + i * 1024, [512], BF16) for i in range(4)]
        RS = carve(f"rs{tmp_off}", tmp_off + 4096, [512], F32)
        for tb in range(ntok // blk):
            ssl = slice(s_t0 + tb * blk, s_t0 + (tb + 1) * blk)
            dsl = slice(d_t0 + tb * blk, d_t0 + (tb + 1) * blk)
            p = psum()
            for c in range(16):
                sq = SQ[c % 4]
                act(sq.ap[:, 0:blk], src.ap[:, c, ssl], AF.Square, [src.res], [sq.res])
                S.mm(p.ap[:, 0:blk], [(ONESB.ap, sq.ap[:, 0:blk])], [sq.res, ONESB.res], [p.res],
                     start=(c == 0), stop=(c == 15))
            rsqrt_from_psum(RS.ap[:, 0:blk], p.ap[:, 0:blk], 1.0 / D, p.res, RS.res)
            for c in range(16):
                S.op("dve", [src.res, RS.res, GAINS.res], [dst.res],
                     lambda e: e.scalar_tensor_tensor(dst.ap[:, c, dsl], src.ap[:, c, ssl],
                                                      GAINS.ap[:, gcol + c:gcol + c + 1], RS.ap[:, 0:blk],
                                                      ALU.mult, ALU.mult))

    def load_fm(dram_rows, dst, ntt, stg):
        for tt in range(ntt):
            st = stg[tt % 2]
            S.dma("sp", f"stg{tt % 2}", [(st.ap, dram_rows(tt))], [], [st.res])
            for cb in range(4):
                p = psum()
                for ci in range(4):
                    c = cb * 4 + ci
                    S.tr(p.ap[:, ci * 128:(ci + 1) * 128], st.ap[:, c * 128:(c + 1) * 128], IDENT.ap,
                         [st.res, IDENT.res], [p.res], signal=(ci == 3))
                ov = dst.ap[:, cb * 4:cb * 4 + 4, tt * 128:(tt + 1) * 128]
                iv = p.ap.rearrange("p (a b) -> p a b", b=128)
                if cb % 2 == 0:
                    act(ov, iv, AF.Copy, [p.res], [dst.res])
                else:
                    S.op("dve", [p.res], [dst.res], lambda e: e.tensor_copy(ov, iv))

    def store_fm(src, row0, ntt, stg):
        for tt in range(ntt):
            st = stg[tt % 2]
            for cb in range(4):
                p = psum()
                for ci in range(4):
                    c = cb * 4 + ci
                    S.tr(p.ap[:, ci * 128:(ci + 1) * 128], src.ap[:, c, tt * 128:(tt + 1) * 128], IDENT.ap,
                         [src.res, IDENT.res], [p.res], signal=(ci == 3))
                ov = st.ap[:, cb * 512:(cb + 1) * 512]
                if cb % 2 == 0:
                    act(ov, p.ap, AF.Copy, [p.res], [st.res])
                else:
                    S.op("dve", [p.res], [st.res], lambda e: e.tensor_copy(ov, p.ap))
            S.dma("sp", f"out{tt % 2}", [(y_d[row0 + tt * 128: row0 + (tt + 1) * 128, :], st.ap)], [st.res], [])

    cparts = [
        (GAINS.ap, gains_d), (GON.ap, gon_d), (IDENT.ap, cst_d[:, 0:128]), (MASKU.ap, cst_d[:, 128:256]),
        (WGU.ap[0:17, :], wgu_d), (BSROW.ap[0:1, :], bs_d), (LNBC.ap, lnbc_d),
    ]
    S.dma("sp", "cst", cparts, [], [GAINS.res, GON.res, IDENT.res, MASKU.res, WGU.res, BSROW.res, LNBC.res])
    S.dma("pool", "wglr", [(WGLR.ap, wsrc(win_d, 0, 16, C_GLR, 16))], [], [WGLR.res])
    S.op("dve", [], [ONESB.res], lambda e: e.memset(ONESB.ap, 1.0))
    S.op("dve", [], [ONESF.res], lambda e: e.memset(ONESF.ap, 1.0))
    for h in range(4):
        S.op("dve", [], [ST[h].res], lambda e: e.memset(ST[h].ap, 0.0))
        S.op("dve", [], [STB[h].res], lambda e: e.memset(STB[h].ap, 0.0))
    S.op("dve", [], [GLR.res], lambda e: e.memset(GLR.ap[0:32, :], 1.0))
    WTMP = carve("WTMP", E_OFF, [512], F32)
    S.dma("sp", "cst2", [(WTMP.ap, wst_d)], [], [WTMP.res])
    for g in range(4):
        S.op("dve", [WTMP.res, MASKU.res], [WST.res],
             lambda e: e.tensor_tensor(WST.ap[:, g, :], WTMP.ap[:, g * 128:(g + 1) * 128], MASKU.ap, ALU.mult))

    S.phase('mem_init')
    MX_ = carve("MEMX", R_OFF, [16, 256], F32)
    MN_ = carve("MEMN", E_OFF + 16384, [16, 256], BF16)
    MSTG = [carve(f"MSTG{i}", E_OFF + i * 8192, [2048], F32) for i in range(2)]
    load_fm(lambda tt: mem_d[tt * 128:(tt + 1) * 128, :], MX_, 2, MSTG)
    rmsnorm(MX_, 0, MN_, 0, 64, 256, HB_OFF, blk=256)
    for cb in range(0, 8, 2):
        wres, (wv,) = wload([wsrc(wkv_d, 0, 16, cb * 128, 256)])
        for ci in range(2):
            p = psum()
            S.mm(p.ap[:, 0:256], [(wv[:, k, ci * 128:(ci + 1) * 128], MN_.ap[:, k, :]) for k in range(16)],
                 [wres, MN_.res], [p.res])
            act(MKT.ap[:, cb + ci, :], p.ap[:, 0:256], AF.Copy, [p.res], [MKT.res])
    for cb2 in range(2):
        banks = [psum() for _ in range(2)]
        for kh in range(2):
            wres, (wv,) = wload([wsrc(wkv_d, kh * 1024, 8, 1024 + cb2 * 512, 512)])
            for mt in range(2):
                p = banks[mt]
                S.mm(p.ap, [(MN_.ap[:, kh * 8 + k, mt * 128:(mt + 1) * 128], wv[:, k, :]) for k in range(8)],
                     [wres, MN_.res], [p.res], start=(kh == 0), stop=(kh == 1))
                if kh == 1:
                    S.op("dve", [p.res], [MV.res], lambda e: e.tensor_copy(MV.ap[:, mt, cb2 * 512:(cb2 + 1) * 512], p.ap))

    def ffn(wg_d, wu_d, wd_d, gcol):
        S.phase('ffn_norm')
        rmsnorm(X, 0, HB, 0, gcol, 1024, R_OFF)
        for g in range(4):
            S.phase(f'ffn_gu{g}')
            for f0 in range(0, 11, 2):
                nf = min(2, 11 - f0)
                c0 = (g * 11 + f0) * 128
                wres, (wgv,) = wload([wsrc(wg_d, 0, 16, c0, nf * 128)])
                for fi in range(nf):
                    for tb in range(2):
                        p = psum()
                        S.mm(p.ap, [(wgv[:, k, fi * 128:(fi + 1) * 128], HB.ap[:, k, tb * 512:(tb + 1) * 512])
                                    for k in range(16)], [wres, HB.res], [p.res])
                        sg = SGT[fi * 2 + tb]
                        act(sg.ap, p.ap, AF.Silu, [p.res], [sg.res])
                wres, (wuv,) = wload([wsrc(wu_d, 0, 16, c0, nf * 128)])
                for fi in range(nf):
                    f = f0 + fi
                    for tb in range(2):
                        p = psum()
                        S.mm(p.ap, [(wuv[:, k, fi * 128:(fi + 1) * 128], HB.ap[:, k, tb * 512:(tb + 1) * 512])
                                    for k in range(16)], [wres, HB.res], [p.res])
                        sg = SGT[fi * 2 + tb]
                        S.op("dve", [sg.res, p.res], [H1.res],
                             lambda e: e.tensor_tensor(H1.ap[:, f, tb * 512:(tb + 1) * 512], sg.ap, p.ap, ALU.mult))
            S.phase(f'ffn_dn{g}')
            for db in range(4):
                banks = [psum() for _ in range(8)]
                for (k0, nk) in ((0, 6), (6, 5)):
                    wres, (wdv,) = wload([wsrc(wd_d, (g * 11 + k0) * 128, nk, db * 512, 512)])
                    for dc in range(4):
                        for tb in range(2):
                            p = banks[dc * 2 + tb]
                            S.mm(p.ap, [(wdv[:, k, dc * 128:(dc + 1) * 128], H1.ap[:, k0 + k, tb * 512:(tb + 1) * 512])
                                        for k in range(nk)], [wres, H1.res], [p.res], start=(k0 == 0), stop=(k0 == 6))
                            if k0 == 6:
                                xv = X.ap[:, db * 4 + dc, tb * 512:(tb + 1) * 512]
                                S.op("dve", [p.res, X.res], [X.res],
                                     lambda e: e.scalar_tensor_tensor(xv, p.ap, 0.5, xv, ALU.mult, ALU.add))

    def fm_proj(col0, nchunks, evac):
        for cb in range(0, nchunks, 2):
            n = min(2, nchunks - cb)
            wres, (wv,) = wload([wsrc(win_d, 0, 16, col0 + cb * 128, n * 128)])
            for ci in range(n):
                p = psum()
                S.mm(p.ap, [(wv[:, k, ci * 128:(ci + 1) * 128], HBS.ap[:, k, :]) for k in range(16)],
                     [wres, HBS.res], [p.res])
                evac(cb + ci, p)

    def tm_proj(col0, evac):
        banks = [psum() for _ in range(4)]
        for kh in range(2):
            wres, (wv,) = wload([wsrc(win_d, kh * 1024, 8, col0, 512)])
            for tt in range(4):
                p = banks[tt]
                S.mm(p.ap, [(HBS.ap[:, kh * 8 + k, tt * 128:(tt + 1) * 128], wv[:, k, :]) for k in range(8)],
                     [wres, HBS.res], [p.res], start=(kh == 0), stop=(kh == 1))
                if kh == 1:
                    evac(tt, p)

    def mixer(t0):
        S.phase('mix_norm')
        rmsnorm(X, t0, HBS, 0, 16, 512, S2)
        S.phase('gla_glr_z')
        p = psum()
        S.mm(p.ap[0:16, :], [(WGLR.ap[:, k, :], HBS.ap[:, k, :]) for k in range(16)], [WGLR.res, HBS.res], [p.res])
        act(GLR.ap[0:16, :], p.ap[0:16, :], AF.Copy, [p.res], [GLR.res])
        for tt in range(4):
            p = psum()
            S.mm(p.ap, [(GLR.ap[0:17, tt * 128:(tt + 1) * 128], WGU.ap[0:17, :])], [GLR.res, WGU.res], [p.res])
            act(LA.ap[:, tt, :], p.ap, AF.Exp, [p.res], [LA.res], scale=-1.0)
            act(LA.ap[:, tt, :], LA.ap[:, tt, :], AF.Ln, [LA.res], [LA.res], bias=1.0)
        S.phase('gla_qk')
        wq_res0, (wq0,) = wload([wsrc(win_d, 0, 8, C_Q, 512)])
        wq_res1, (wq1,) = wload([wsrc(win_d, 1024, 8, C_Q, 512)])

        def cumT(h):
            pb = psum()
            for c in range(4):
                S.mm(pb.ap[:, c * 128:(c + 1) * 128], [(LA.ap[:, c, h * 128:(h + 1) * 128], MASKU.ap)],
                     [LA.res, MASKU.res], [pb.res], signal=(c == 3))
            return pb

        for h in range(4):
            pb = cumT(h)
            eb = EBT[h % 2]
            act(eb.ap, pb.ap, AF.Exp, [pb.res], [eb.res], scale=-1.0 / 16)
            S.op("dve", [eb.res], [EBL.res], lambda e: e.tensor_copy(EBL.ap[:, h * 4:(h + 1) * 4], eb.ap[:, 127:512:128]))
            pq = psum()
            hs = slice(h * 128, (h + 1) * 128)
            S.mm(pq.ap, [(wq0[:, k, hs], HBS.ap[:, k, :]) for k in range(8)] + [(wq1[:, k, hs], HBS.ap[:, 8 + k, :]) for k in range(8)],
                 [wq_res0, wq_res1, HBS.res], [pq.res])
            S.op("dve", [pq.res, eb.res], [QT.res],
                 lambda e: e.scalar_tensor_tensor(QT.ap[:, h, :], pq.ap, 128 ** -0.5, eb.ap, ALU.mult, ALU.mult))
        wk_res0, (wk0,) = wload([wsrc(win_d, 0, 8, C_K, 512)])
        wk_res1, (wk1,) = wload([wsrc(win_d, 1024, 8, C_K, 512)])
        for h in range(4):
            pb = cumT(h)
            enb = ENBT[h % 2]
            act(enb.ap, pb.ap, AF.Exp, [pb.res], [enb.res], scale=1.0 / 16)
            pk = psum()
            hs = slice(h * 128, (h + 1) * 128)
            S.mm(pk.ap, [(wk0[:, k, hs], HBS.ap[:, k, :]) for k in range(8)] + [(wk1[:, k, hs], HBS.ap[:, 8 + k, :]) for k in range(8)],
                 [wk_res0, wk_res1, HBS.res], [pk.res])
            S.op("dve", [pk.res, enb.res], [KT.res], lambda e: e.tensor_tensor(KT.ap[:, h, :], pk.ap, enb.ap, ALU.mult))
        S.phase('gla_ktm')
        for c in range(4):
            pbt = psum()
            S.mm(pbt.ap, [(MASKU.ap, LA.ap[:, c, :])], [LA.res, MASKU.res], [pbt.res])
            act(ENBTM.ap, pbt.ap, AF.Exp, [pbt.res], [ENBTM.res], scale=1.0 / 16)
            pk = psum()
            csl_ = slice(c * 128, (c + 1) * 128)
            S.mm(pk.ap, [(HBS.ap[:, k, csl_], wk0[:, k, :]) for k in range(8)] + [(HBS.ap[:, 8 + k, csl_], wk1[:, k, :]) for k in range(8)],
                 [wk_res0, wk_res1, HBS.res], [pk.res])
            S.op("dve", [pk.res, ENBTM.res], [KTM.res], lambda e: e.tensor_tensor(KTM.ap[:, c, :], pk.ap, ENBTM.ap, ALU.mult))
        S.phase('gla_v')
        for cb2 in range(2):
            tm_proj(C_V + cb2 * 512,
                    lambda tt, p: act(VTM.ap[:, tt, cb2 * 512:(cb2 + 1) * 512], p.ap, AF.Copy, [p.res], [VTM.res]))
        S.phase('gla_r')
        fm_proj(C_R, 8, lambda ch, p: act(YGLA.ap[:, ch, :], p.ap, AF.Silu, [p.res], [YGLA.res]))
        S.phase('gla_loop')
        it = 0
        for c in range(4):
            csl = slice(c * 128, (c + 1) * 128)
            for h in range(4):
                i2 = it % 2
                it += 1
                ps_ = psum()
                S.mm(ps_.ap[:, 0:128], [(KT.ap[:, h, csl], QT.ap[:, h, csl])], [KT.res, QT.res], [ps_.res])
                pt = PTs[i2]
                S.op("dve", [ps_.res, MASKU.res], [pt.res], lambda e: e.tensor_tensor(pt.ap, ps_.ap[:, 0:128], MASKU.ap, ALU.mult))
                po = psum()
                for j in range(2):
                    S.mm(po.ap[:, j * 128:(j + 1) * 128],
                         [(VTM.ap[:, c, h * 256 + j * 128:h * 256 + (j + 1) * 128], pt.ap),
                          (STB[h].ap[:, j * 128:(j + 1) * 128], QT.ap[:, h, csl])],
                         [VTM.res, pt.res, STB[h].res, QT.res], [po.res], signal=(j == 1))
                pkv = psum()
                S.mm(pkv.ap[:, 0:256], [(KTM.ap[:, c, h * 128:(h + 1) * 128], VTM.ap[:, c, h * 256:(h + 1) * 256])],
                     [KTM.res, VTM.res], [pkv.res])
                ebl = EBL.ap[:, h * 4 + c:h * 4 + c + 1]
                S.op("dve", [ST[h].res, EBL.res], [ST[h].res], lambda e: e.tensor_scalar(ST[h].ap, ST[h].ap, ebl, None, ALU.mult))
                S.op("dve", [pkv.res, ST[h].res, EBL.res], [ST[h].res],
                     lambda e: e.scalar_tensor_tensor(ST[h].ap, pkv.ap[:, 0:256], ebl, ST[h].ap, ALU.mult, ALU.add))
                act(STB[h].ap, ST[h].ap, AF.Copy, [ST[h].res], [STB[h].res])
                osq = OSQ[i2]
                act(osq.ap, po.ap[:, 0:256], AF.Square, [po.res], [osq.res])
                pss = psum()
                S.mm(pss.ap[:, 0:128], [(ONESB.ap, osq.ap[:, 0:128]), (ONESB.ap, osq.ap[:, 128:256])],
                     [ONESB.res, osq.res], [pss.res])
                rs = RSG[i2]
                rsqrt_from_psum(rs.ap, pss.ap[:, 0:128], 1.0 / 256, pss.res, rs.res)
                ot = OT[i2]
                for j in range(2):
                    S.op("dve", [po.res, rs.res, GON.res], [ot.res],
                         lambda e: e.scalar_tensor_tensor(ot.ap[:, j, :], po.ap[:, j * 128:(j + 1) * 128],
                                                          GON.ap[:, h * 2 + j:h * 2 + j + 1], rs.ap, ALU.mult, ALU.mult))
                yv = YGLA.ap[:, h * 2:h * 2 + 2, csl]
                S.op("dve", [ot.res, YGLA.res], [YGLA.res], lambda e: e.tensor_tensor(yv, ot.ap, yv, ALU.mult))
        S.phase('sg_u')
        fm_proj(C_SU, 8, lambda ch, p: act(YSG.ap[:, ch, :], p.ap, AF.Gelu, [p.res], [YSG.res]))
        S.phase('sg_v')
        it = 0
        for cb2 in range(2):
            def sv_evac(tt, p, cb2=cb2):
                gv = GV[tt % 2]
                act(gv.ap, p.ap, AF.Gelu, [p.res], [gv.res])
                for gi in range(2):
                    S.op("dve", [gv.res], [BNS.res], lambda e: e.bn_stats(BNS.ap[:, gi, :], gv.ap[:, gi * 256:(gi + 1) * 256]))
                    S.op("dve", [BNS.res], [BNA.res], lambda e: e.bn_aggr(BNA.ap[:, gi, :], BNS.ap[:, gi, :]))
                act(LNR.ap, BNA.ap[:, :, 1], AF.Ln, [BNA.res], [LNR.res], bias=EPS)
                act(LNR.ap, LNR.ap, AF.Exp, [LNR.res], [LNR.res], scale=-0.5)
                for gi in range(2):
                    gsl = slice(gi * 256, (gi + 1) * 256)
                    S.op("dve", [gv.res, BNA.res, LNR.res], [gv.res],
                         lambda e: e.tensor_scalar(gv.ap[:, gsl], gv.ap[:, gsl], BNA.ap[:, gi, 0:1], LNR.ap[:, gi:gi + 1],
                                                   ALU.subtract, ALU.mult))
                lsl = slice(cb2 * 512, (cb2 + 1) * 512)
                S.op("dve", [gv.res, LNBC.res], [gv.res], lambda e: e.tensor_tensor(gv.ap, gv.ap, LNBC.ap[:, lsl], ALU.mult))
                S.op("dve", [gv.res, LNBC.res], [SVTM.res],
                     lambda e: e.tensor_tensor(SVTM.ap[:, tt, lsl], gv.ap, LNBC.ap[:, 1024 + cb2 * 512:1024 + (cb2 + 1) * 512], ALU.add))
            tm_proj(C_SV + cb2 * 512, sv_evac)
        S.phase('sg_mix')
        for g in range(4):
            for cj in range(2):
                p = psum()
                for tt in range(4):
                    S.mm(p.ap[:, tt * 128:(tt + 1) * 128],
                         [(SVTM.ap[:, tt, g * 256 + cj * 128:g * 256 + (cj + 1) * 128], WST.ap[:, g, :]),
                          (ONESF.ap[0:1, :], BSROW.ap[0:1, g * 128:(g + 1) * 128])],
                         [SVTM.res, WST.res, ONESF.res, BSROW.res], [p.res], signal=(tt == 3))
                yv = YSG.ap[:, g * 2 + cj, :]
                S.op("dve", [p.res, YSG.res], [YSG.res], lambda e: e.tensor_tensor(yv, p.ap, yv, ALU.mult))
        S.phase('xa_q')
        fm_proj(C_XQ, 8, lambda ch, p: act(XQT.ap[:, ch, :], p.ap, AF.Copy, [p.res], [XQT.res], scale=1.0 / 16))
        S.phase('xa_attn')
        for tt in range(4):
            tsl = slice(tt * 128, (tt + 1) * 128)
            pf, pt2 = PF[tt % 2], PT2[tt % 2]
            psc = [psum(), psum()]
            for h in range(4):
                S.mm(psc[h // 2].ap[:, (h % 2) * 256:(h % 2 + 1) * 256],
                     [(XQT.ap[:, h * 2 + j, tsl], MKT.ap[:, h * 2 + j, :]) for j in range(2)],
                     [XQT.res, MKT.res], [psc[h // 2].res], signal=(h % 2 == 1))
            for b in range(2):
                S.op("dve", [psc[b].res], [MXB.res],
                     lambda e: e.tensor_reduce(MXB.ap[:, 2 * b:2 * b + 2], psc[b].ap.rearrange("p (h m) -> p h m", m=256), AX.X, ALU.max))
            S.op("dve", [MXB.res], [NMX.res], lambda e: e.tensor_scalar(NMX.ap, MXB.ap, -1.0, None, ALU.mult))
            for h in range(4):
                act(pf.ap[:, h, :], psc[h // 2].ap[:, (h % 2) * 256:(h % 2 + 1) * 256], AF.Exp,
                    [psc[h // 2].res, NMX.res], [pf.res, RSUM.res], bias=NMX.ap[:, h:h + 1], accum_out=RSUM.ap[:, h:h + 1])
            S.op("dve", [RSUM.res], [RINV.res], lambda e: e.reciprocal(RINV.ap, RSUM.ap))
            for h in range(4):
                S.op("dve", [pf.res, RINV.res], [pf.res],
                     lambda e: e.tensor_scalar(pf.ap[:, h, :], pf.ap[:, h, :], RINV.ap[:, h:h + 1], None, ALU.mult))
            for b in range(2):
                ptp = psum()
                for q in range(4):
                    h, mt = b * 2 + q // 2, q % 2
                    S.tr(ptp.ap[:, q * 128:(q + 1) * 128], pf.ap[:, h, mt * 128:(mt + 1) * 128], IDENT.ap,
                         [pf.res, IDENT.res], [ptp.res], signal=(q == 3))
                ov = pt2.ap[:, b * 4:(b + 1) * 4, :]
                iv = ptp.ap.rearrange("p (a b) -> p a b", b=128)
                if b == 0:
                    act(ov, iv, AF.Copy, [ptp.res], [pt2.res])
                else:
                    S.op("dve", [ptp.res], [pt2.res], lambda e: e.tensor_copy(ov, iv))
            for b in range(2):
                po = psum()
                for q in range(4):
                    hj = b * 4 + q
                    h, j = hj // 2, hj % 2
                    S.mm(po.ap[:, q * 128:(q + 1) * 128],
                         [(MV.ap[:, mt, h * 256 + j * 128:h * 256 + (j + 1) * 128], pt2.ap[:, h * 2 + mt, :]) for mt in range(2)],
                         [MV.res, pt2.res], [po.res], signal=(q == 3))
                ov = YXA.ap[:, b * 4:(b + 1) * 4, tsl]
                iv = po.ap.rearrange("p (a b) -> p a b", b=128)
                if b == 0:
                    act(ov, iv, AF.Copy, [po.res], [YXA.res])
                else:
                    S.op("dve", [po.res], [YXA.res], lambda e: e.tensor_copy(ov, iv))
        ys = [YGLA, YSG, YXA]
        S.phase('merge')
        for jg in range(4):
            mrg = MRG[jg % 2]
            for i in range(3):
                gt = GT[i % 2]
                for half in range(2):
                    wres, (wv,) = wload([wsrc(win_d, 0, 16, C_GATE + i * 2048 + jg * 512 + half * 256, 256)])
                    for j2 in range(2):
                        ji = half * 2 + j2
                        p = psum()
                        S.mm(p.ap, [(wv[:, k, j2 * 128:(j2 + 1) * 128], HBS.ap[:, k, :]) for k in range(16)], [wres, HBS.res], [p.res])
                        act(gt.ap[:, ji, :], p.ap, AF.Sigmoid, [p.res], [gt.res])
                wres, (wv,) = wload([wsrc(wbr_d, i * 1024, 8, jg * 512, 512)])
                for ji in range(4):
                    p = psum()
                    S.mm(p.ap, [(wv[:, k, ji * 128:(ji + 1) * 128], ys[i].ap[:, k, :]) for k in range(8)], [wres, ys[i].res], [p.res])
                    if i == 0:
                        S.op("dve", [p.res, gt.res], [ACC.res], lambda e: e.tensor_tensor(ACC.ap[:, ji, :], gt.ap[:, ji, :], p.ap, ALU.mult))
                    else:
                        tt_ = TT_[ji % 2]
                        S.op("dve", [p.res, gt.res], [tt_.res], lambda e: e.tensor_tensor(tt_.ap, gt.ap[:, ji, :], p.ap, ALU.mult))
                        if i == 1:
                            S.op("dve", [tt_.res, ACC.res], [ACC.res], lambda e: e.tensor_tensor(ACC.ap[:, ji, :], ACC.ap[:, ji, :], tt_.ap, ALU.add))
                        else:
                            S.op("dve", [tt_.res, ACC.res], [mrg.res], lambda e: e.tensor_tensor(mrg.ap[:, ji, :], ACC.ap[:, ji, :], tt_.ap, ALU.add))
            for oh in range(2):
                wres, (wv,) = wload([wsrc(wout_d, jg * 512, 4, oh * 1024, 1024)])
                for d8 in range(8):
                    dc = oh * 8 + d8
                    p = psum()
                    S.mm(p.ap, [(wv[:, k, d8 * 128:(d8 + 1) * 128], mrg.ap[:, k, :]) for k in range(4)], [wres, mrg.res], [p.res])
                    xv = X.ap[:, dc, t0:t0 + 512]
                    S.op("dve", [p.res, X.res], [X.res], lambda e: e.tensor_tensor(xv, p.ap, xv, ALU.add))

    for tile in range(2):
        row0 = tile * 1024
        S.phase('load_x')
        load_fm(lambda tt: x_d[row0 + tt * 128: row0 + (tt + 1) * 128, :], X, 8, STG)
        ffn(*ffw[0], 0)
        for sub in range(2):
            mixer(sub * 512)
        ffn(*ffw[1], 32)
        S.phase('final_norm')
        rmsnorm(X, 0, X, 0, 48, 1024, E_OFF)
        S.phase('store')
        store_fm(X, row0, 8, STG)
    for k in ("out0", "out1"):
        nc.sync.wait_ge(S.sem[k], S.cnt[k])
    S.phase('end')
    nc._phases = S.phases
    return nc


_NC_CACHE = {}


def _fm(v):
    return np.ascontiguousarray(np.asarray(v, np.float32).reshape(16, 128).T)


def kernel(x, mem, ffn1_norm, ffn1_w_gate, ffn1_w_up, ffn1_w_down, mix_norm, mem_norm,
           w_in, gla_w_gate_up, gla_gate_bias, gla_out_norm, sg_ln_g, sg_ln_b, sg_w_s, sg_b_s,
           w_kv_mem, w_branch, w_out, ffn2_norm, ffn2_w_gate, ffn2_w_up, ffn2_w_down, final_norm):
    f = lambda a: np.ascontiguousarray(np.asarray(a, np.float32))
    x = f(x)
    mem = f(mem)
    B = x.shape[0]
    gains = np.concatenate([_fm(f(ffn1_norm)[0]), _fm(f(mix_norm)[0]), _fm(f(ffn2_norm)[0]),
                            _fm(f(final_norm)), _fm(f(mem_norm)[0])], axis=1)
    gon = np.ascontiguousarray(f(gla_out_norm)[0].reshape(8, 128).T)
    lnbc = np.ascontiguousarray(np.broadcast_to(
        np.concatenate([f(sg_ln_g)[0].reshape(-1), f(sg_ln_b)[0].reshape(-1)])[None, :], (128, 2048)))
    wst = np.ascontiguousarray(f(sg_w_s)[0].transpose(2, 0, 1).reshape(128, 512))
    wgu = np.ascontiguousarray(np.concatenate([f(gla_w_gate_up)[0], f(gla_gate_bias)[0][None, :]], axis=0))
    bs = np.ascontiguousarray(f(sg_b_s)[0].reshape(1, 512))
    cst = np.ascontiguousarray(np.concatenate([np.eye(128, dtype=np.float32),
                                               np.triu(np.ones((128, 128), np.float32))], axis=1))
    shared = {
        "w_in": f(w_in)[0], "f1g": f(ffn1_w_gate)[0], "f1u": f(ffn1_w_up)[0], "f1d": f(ffn1_w_down)[0],
        "f2g": f(ffn2_w_gate)[0], "f2u": f(ffn2_w_up)[0], "f2d": f(ffn2_w_down)[0],
        "w_kv": f(w_kv_mem)[0], "w_br": f(w_branch)[0].reshape(3072, 2048), "w_out": f(w_out)[0],
        "gains": gains, "gon": gon, "lnbc": lnbc, "wst": wst, "wgu": wgu, "bs": bs, "cst": cst,
    }
    if "nc" not in _NC_CACHE:
        _NC_CACHE["nc"] = build_nc()
    nc = _NC_CACHE["nc"]
    in_maps = []
    for b in range(B):
        m = dict(shared)
        m["x"] = x[b]
        m["mem"] = mem[b]
        in_maps.append(m)
    res = run_bass_kernel_spmd(nc, in_maps, core_ids=list(range(B)))
    return np.stack([np.asarray(r["y"], np.float32) for r in res.results], axis=0)
```

```python
import numpy as np
import concourse.bass as bass
import concourse.mybir as mybir
from concourse.bass_utils import run_bass_kernel_spmd

F32 = mybir.dt.float32
BF16 = mybir.dt.bfloat16
AF = mybir.ActivationFunctionType
ALU = mybir.AluOpType
AX = mybir.AxisListType

D = 2048
SEQ = 2048
DFF = 5632
EPS = 1e-6
C_Q, C_K, C_V, C_R, C_GLR, C_SU, C_SV, C_XQ, C_GATE = 0, 512, 1024, 2048, 3072, 3088, 4112, 5136, 6160


class Res:
    __slots__ = ("name", "lo", "hi", "w", "r", "al")

    def __init__(self, name, lo=None, hi=None):
        self.name, self.lo, self.hi = name, lo, hi
        self.w = None
        self.r = {}
        self.al = []


class Buf:
    __slots__ = ("ap", "res")

    def __init__(self, ap, res):
        self.ap, self.res = ap, res


class Sched:
    def __init__(self, nc):
        self.nc = nc
        self.eng = {"pe": nc.tensor, "act": nc.scalar, "dve": nc.vector, "pool": nc.gpsimd, "sp": nc.sync}
        self.sem = {}
        self.cnt = {}
        self.seen = {e: {} for e in self.eng}
        for e in self.eng:
            self.sem[e] = nc.alloc_semaphore("s_" + e)
            self.cnt[e] = 0
        self.all_res = []
        self.npe = 0
        self.phases = []

    def phase(self, name):
        self.phases.append((name, self.npe))

    def _sync(self, e, reads, writes):
        need = {}

        def add(ev):
            if ev is None:
                return
            k, v = ev
            if need.get(k, 0) < v:
                need[k] = v

        for r in reads:
            add(r.w)
            for t in r.al:
                add(t.w)
        for w in writes:
            for t in [w] + w.al:
                for k, v in t.r.items():
                    add((k, v))
                add(t.w)
        seen = self.seen[e]
        eng = self.eng[e]
        for k, v in need.items():
            if e == "pe" and k == "pe":
                continue
            if seen.get(k, 0) >= v:
                continue
            eng.wait_ge(self.sem[k], v)
            seen[k] = v

    def _post(self, key, val, reads, writes):
        for r in reads:
            r.r[key] = val
        for w in writes:
            w.w = (key, val)
            w.r = {}

    def op(self, e, reads, writes, fn):
        self._sync(e, reads, writes)
        ins = fn(self.eng[e])
        self.cnt[e] += 1
        ins.then_inc(self.sem[e], 1)
        self._post(e, self.cnt[e], reads, writes)

    def mm(self, out_ap, pairs, reads, writes, start=True, stop=True, signal=True):
        self._sync("pe", reads, writes)
        n = len(pairs)
        ins = None
        self.npe += n
        for i, (l, r) in enumerate(pairs):
            ins = self.nc.tensor.matmul(out_ap, lhsT=l, rhs=r, start=(start and i == 0), stop=(stop and i == n - 1))
        if signal:
            self.cnt["pe"] += 1
            ins.then_inc(self.sem["pe"], 1)
            self._post("pe", self.cnt["pe"], reads, writes)
        else:
            self._post("pe", self.cnt["pe"] + 1, reads, writes)

    def tr(self, out_ap, in_ap, ident_ap, reads, writes, signal=True):
        self._sync("pe", reads, writes)
        self.npe += 1
        ins = self.nc.tensor.transpose(out_ap, in_ap, ident_ap)
        if signal:
            self.cnt["pe"] += 1
            ins.then_inc(self.sem["pe"], 1)
            self._post("pe", self.cnt["pe"], reads, writes)
        else:
            self._post("pe", self.cnt["pe"] + 1, reads, writes)

    def dma(self, q, key, parts, reads, writes):
        if key not in self.sem:
            self.sem[key] = self.nc.alloc_semaphore("d_" + key)
            self.cnt[key] = 0
        self._sync(q, reads, writes)
        for (o, i) in parts:
            ins = self.eng[q].dma_start(out=o, in_=i)
            self.cnt[key] += 16
            ins.then_inc(self.sem[key], 16)
        self._post(key, self.cnt[key], reads, writes)


def build_nc():
    nc = bass.Bass("TRN2", target_bir_lowering=False)
    S = Sched(nc)

    def din(name, shape):
        return nc.dram_tensor(name, list(shape), F32, kind="ExternalInput").ap()

    x_d = din("x", [SEQ, D])
    mem_d = din("mem", [256, D])
    win_d = din("w_in", [D, 12304])
    ffw = []
    for i in (1, 2):
        ffw.append((din(f"f{i}g", [D, DFF]), din(f"f{i}u", [D, DFF]), din(f"f{i}d", [DFF, D])))
    wkv_d = din("w_kv", [D, D])
    wbr_d = din("w_br", [3072, D])
    wout_d = din("w_out", [D, D])
    gains_d = din("gains", [128, 80])
    gon_d = din("gon", [128, 8])
    lnbc_d = din("lnbc", [128, 2048])
    wst_d = din("wst", [128, 512])
    wgu_d = din("wgu", [17, 512])
    bs_d = din("bs", [1, 512])
    cst_d = din("cst", [128, 256])
    y_d = nc.dram_tensor("y", [SEQ, D], F32, kind="ExternalOutput").ap()

    TOTAL = 212480
    BIG = nc.alloc_sbuf_tensor("BIG", [128, TOTAL // 2], BF16)

    def carve(name, off, shape, dtype):
        esz = 2 if dtype == BF16 else 4
        n = 1
        for s_ in shape:
            n *= s_
        assert off % 4 == 0 and off + n * esz <= TOTAL, (name, off, n * esz)
        ap = BIG[:, off // 2: off // 2 + n * esz // 2]
        if dtype == F32:
            ap = ap.bitcast(F32)
        if len(shape) == 2:
            ap = ap.rearrange("p (a b) -> p a b", b=shape[1])
        res = Res(name, off, off + n * esz)
        for o in S.all_res:
            if o.lo < res.hi and res.lo < o.hi:
                o.al.append(res)
                res.al.append(o)
        S.all_res.append(res)
        return Buf(ap, res)

    X_OFF = 0
    HB_OFF = 65536
    W_OFF = 98304
    R_OFF = 131072
    E_OFF = 153600
    BS_OFF = 180224
    C_OFF = 182272
    MKT_OFF = 196608
    MV_OFF = 200704
    ST_OFF = 204800
    STB_OFF = 208896
    SM_OFF = 210944
    S1 = R_OFF
    S2 = HB_OFF + 16384

    X = carve("X", X_OFF, [16, 1024], F32)
    HB = carve("HB", HB_OFF, [16, 1024], BF16)
    HBS = carve("HBS", HB_OFF, [16, 512], BF16)
    WS = [carve(f"W{i}", W_OFF + i * 8192, [4096], BF16) for i in range(4)]
    H1 = carve("H1", R_OFF, [11, 1024], BF16)
    STG = [carve(f"STG{i}", R_OFF + i * 8192, [2048], F32) for i in range(2)]
    SGT = [carve(f"SGT{i}", E_OFF + i * 1024, [512], BF16) for i in range(4)]
    o = C_OFF
    GAINS = carve("GAINS", o, [80], F32); o += 320
    GON = carve("GON", o, [8], F32); o += 32
    IDENT = carve("IDENT", o, [128], F32); o += 512
    MASKU = carve("MASKU", o, [128], F32); o += 512
    ONESB = carve("ONESB", o, [128], BF16); o += 256
    ONESF = carve("ONESF", o, [128], F32); o += 512
    WGU = carve("WGU", o, [512], F32); o += 2048
    WST = carve("WST", o, [4, 128], BF16); o += 1024
    WGLR = carve("WGLR", o, [16, 16], BF16); o += 512
    LNBC = carve("LNBC", o, [2048], F32); o += 8192
    assert o <= C_OFF + 14336
    BSROW = carve("BSROW", BS_OFF, [512], F32)
    MKT = carve("MKT", MKT_OFF, [8, 256], BF16)
    MV = carve("MV", MV_OFF, [2, 1024], BF16)
    ST = [carve(f"ST{h}", ST_OFF + h * 1024, [256], F32) for h in range(4)]
    STB = [carve(f"STB{h}", STB_OFF + h * 512, [256], BF16) for h in range(4)]
    o = SM_OFF
    EBL = carve("EBL", o, [16], F32); o += 64
    BNS = carve("BNS", o, [2, 6], F32); o += 48
    BNA = carve("BNA", o, [2, 2], F32); o += 16
    LNR = carve("LNR", o, [2], F32); o += 8
    o += 8
    MXB = carve("MXB", o, [4], F32); o += 16
    NMX = carve("NMX", o, [4], F32); o += 16
    RSUM = carve("RSUM", o, [4], F32); o += 16
    RINV = carve("RINV", o, [4], F32); o += 16
    assert o <= TOTAL
    QT = carve("QT", S1 + 0, [4, 512], BF16)
    KT = carve("KT", S1 + 4096, [4, 512], BF16)
    KTM = carve("KTM", S1 + 8192, [4, 512], BF16)
    VTM = carve("VTM", S1 + 12288, [4, 1024], BF16)
    GLR = carve("GLR", S1 + 20480, [512], F32)
    LA = carve("LA", S2 + 0, [4, 512], F32)
    EBT = [carve(f"EBT{i}", S2 + 8192 + i * 2048, [512], F32) for i in range(2)]
    ENBT = [carve(f"ENBT{i}", S2 + 12288 + i * 2048, [512], F32) for i in range(2)]
    YGLA = carve("YGLA", E_OFF, [8, 512], BF16)
    YSG = carve("YSG", E_OFF + 8192, [8, 512], BF16)
    YXA = carve("YXA", E_OFF + 16384, [8, 512], BF16)
    ENBTM = carve("ENBTM", E_OFF + 24576, [512], F32)
    PT4 = [carve(f"PT4_{i}", S2 + i * 1024, [4, 128], BF16) for i in range(2)]
    OSQ8 = carve("OSQ8", S2 + 2048, [8, 128], BF16)
    RS4 = carve("RS4", S2 + 4096, [4, 128], F32)
    OTT = carve("OTT", S2 + 6144, [8, 128], F32)
    RTMP = [carve(f"RTMP{i}", S2 + 10240 + i * 2048, [512], F32) for i in range(2)]
    MASK4 = carve("MASK4", SM_OFF + 256, [4, 128], BF16)
    ST4 = carve("ST4", ST_OFF, [4, 256], F32)
    STB4 = carve("STB4", STB_OFF, [4, 256], BF16)
    GV4 = carve("GV4", S2, [4, 512], F32)
    BNS8 = carve("BNS8", SM_OFF + 1280, [8, 6], F32)
    BNA8 = carve("BNA8", SM_OFF + 1280 + 192, [8, 2], F32)
    LNR8 = carve("LNR8", SM_OFF + 208, [8], F32)
    SVTM = carve("SVTM", S1 + 0, [4, 1024], BF16)
    XQT = carve("XQT", S1 + 8192, [8, 512], BF16)
    PF = [carve(f"PF{i}", S2 + 8192 + i * 4096, [4, 256], F32) for i in range(2)]
    PT2 = [carve(f"PT2{i}", S1 + 16384 + i * 2048, [8, 128], BF16) for i in range(2)]
    ACC = carve("ACC", S2 + 0, [4, 512], F32)
    GT = [carve(f"GT{i}", S2 + 8192 + i * 4096, [4, 512], BF16) for i in range(2)]
    MRG = [carve(f"MRG{i}", S1 + i * 4096, [4, 512], BF16) for i in range(2)]
    TT_ = [carve(f"TT{i}", S1 + 8192 + i * 2048, [512], F32) for i in range(2)]

    PS = []
    for i in range(8):
        t = nc.alloc_psum_tensor(f"ps{i}", [128, 512], F32)
        PS.append(Buf(t[:, :], Res(f"ps{i}")))
    state = {"pi": 0, "wi": 0}

    def psum():
        b = PS[state["pi"] % 8]
        state["pi"] += 1
        return b

    def wsrc(d2, r0, nk, c0, ncol):
        return d2[r0:r0 + nk * 128, c0:c0 + ncol].rearrange("(k p) c -> p k c", p=128)

    def wload(srcs):
        idx = state["wi"] % 4
        state["wi"] += 1
        slot = WS[idx]
        off = 0
        views, parts = [], []
        for s_ in srcs:
            nk, ncol = s_.shape[1], s_.shape[2]
            v = slot.ap[:, off:off + nk * ncol].rearrange("p (k c) -> p k c", c=ncol)
            parts.append((v, s_))
            views.append(v)
            off += nk * ncol
        assert off <= 4096
        S.dma("pool", f"w{idx}", parts, [], [slot.res])
        return slot.res, views

    def act(out, in_, func, reads, writes, **kw):
        S.op("act", reads, writes, lambda e: e.activation(out, in_, func, **kw))

    def rsqrt_from_psum(dst, ps_ap, scale, reads_ps, dres):
        act(dst, ps_ap, AF.Ln, [reads_ps], [dres], bias=EPS, scale=scale)
        act(dst, dst, AF.Exp, [dres], [dres], scale=-0.5)

    def rmsnorm(src, s_t0, dst, d_t0, gcol, ntok, tmp_off, blk=512):
        SQ = [carve(f"sq{tmp_off}_{i}", tmp_off + i * 1024, [512], BF16) for i in range(4)]
        RS = carve(f"rs{tmp_off}", tmp_off + 4096, [512], F32)
        for tb in range(ntok // blk):
            ssl = slice(s_t0 + tb * blk, s_t0 + (tb + 1) * blk)
            dsl = slice(d_t0 + tb * blk, d_t0 + (tb + 1) * blk)
            p = psum()
            for c in range(16):
                sq = SQ[c % 4]
                if c % 2 == 0:
                    act(sq.ap[:, 0:blk], src.ap[:, c, ssl], AF.Square, [src.res], [sq.res])
                else:
                    S.op("dve", [src.res], [sq.res],
                         lambda e: e.tensor_tensor(sq.ap[:, 0:blk], src.ap[:, c, ssl], src.ap[:, c, ssl], ALU.mult))
                S.mm(p.ap[:, 0:blk], [(ONESB.ap, sq.ap[:, 0:blk])], [sq.res, ONESB.res], [p.res],
                     start=(c == 0), stop=(c == 15))
            rsqrt_from_psum(RS.ap[:, 0:blk], p.ap[:, 0:blk], 1.0 / D, p.res, RS.res)
            for c in range(16):
                S.op("dve", [src.res, RS.res, GAINS.res], [dst.res],
                     lambda e: e.scalar_tensor_tensor(dst.ap[:, c, dsl], src.ap[:, c, ssl],
                                                      GAINS.ap[:, gcol + c:gcol + c + 1], RS.ap[:, 0:blk],
                                                      ALU.mult, ALU.mult))

    def load_fm(dram_rows, dst, ntt, stg):
        for tt in range(ntt):
            st = stg[tt % 2]
            S.dma("sp", f"stg{tt % 2}", [(st.ap, dram_rows(tt))], [], [st.res])
            for cb in range(4):
                p = psum()
                for ci in range(4):
                    c = cb * 4 + ci
                    S.tr(p.ap[:, ci * 128:(ci + 1) * 128], st.ap[:, c * 128:(c + 1) * 128], IDENT.ap,
                         [st.res, IDENT.res], [p.res], signal=(ci == 3))
                ov = dst.ap[:, cb * 4:cb * 4 + 4, tt * 128:(tt + 1) * 128]
                iv = p.ap.rearrange("p (a b) -> p a b", b=128)
                if cb % 2 == 0:
                    act(ov, iv, AF.Copy, [p.res], [dst.res])
                else:
                    S.op("dve", [p.res], [dst.res], lambda e: e.tensor_copy(ov, iv))

    def store_fm(src, row0, ntt, stg):
        for tt in range(ntt):
            st = stg[tt % 2]
            for cb in range(4):
                p = psum()
                for ci in range(4):
                    c = cb * 4 + ci
                    S.tr(p.ap[:, ci * 128:(ci + 1) * 128], src.ap[:, c, tt * 128:(tt + 1) * 128], IDENT.ap,
                         [src.res, IDENT.res], [p.res], signal=(ci == 3))
                ov = st.ap[:, cb * 512:(cb + 1) * 512]
                if cb % 2 == 0:
                    act(ov, p.ap, AF.Copy, [p.res], [st.res])
                else:
                    S.op("dve", [p.res], [st.res], lambda e: e.tensor_copy(ov, p.ap))
            S.dma("sp", f"out{tt % 2}", [(y_d[row0 + tt * 128: row0 + (tt + 1) * 128, :], st.ap)], [st.res], [])

    cparts = [
        (GAINS.ap, gains_d), (GON.ap, gon_d), (IDENT.ap, cst_d[:, 0:128]), (MASKU.ap, cst_d[:, 128:256]),
        (WGU.ap[0:17, :], wgu_d), (BSROW.ap[0:1, :], bs_d), (LNBC.ap, lnbc_d),
    ]
    S.dma("sp", "cst", cparts, [], [GAINS.res, GON.res, IDENT.res, MASKU.res, WGU.res, BSROW.res, LNBC.res])
    S.dma("pool", "wglr", [(WGLR.ap, wsrc(win_d, 0, 16, C_GLR, 16))], [], [WGLR.res])
    S.op("dve", [], [ONESB.res], lambda e: e.memset(ONESB.ap, 1.0))
    S.op("dve", [], [ONESF.res], lambda e: e.memset(ONESF.ap, 1.0))
    for h in range(4):
        S.op("dve", [], [ST[h].res], lambda e: e.memset(ST[h].ap, 0.0))
        S.op("dve", [], [STB[h].res], lambda e: e.memset(STB[h].ap, 0.0))
    S.op("dve", [], [GLR.res], lambda e: e.memset(GLR.ap[0:32, :], 1.0))
    WTMP = carve("WTMP", E_OFF, [512], F32)
    S.dma("sp", "cst2", [(WTMP.ap, wst_d)], [], [WTMP.res])
    for g in range(4):
        S.op("dve", [WTMP.res, MASKU.res], [WST.res],
             lambda e: e.tensor_tensor(WST.ap[:, g, :], WTMP.ap[:, g * 128:(g + 1) * 128], MASKU.ap, ALU.mult))

    for h in range(4):
        S.op("dve", [MASKU.res], [MASK4.res], lambda e: e.tensor_copy(MASK4.ap[:, h, :], MASKU.ap))
    S.phase('mem_init')
    MX_ = carve("MEMX", R_OFF, [16, 256], F32)
    MN_ = carve("MEMN", E_OFF + 16384, [16, 256], BF16)
    MSTG = [carve(f"MSTG{i}", E_OFF + i * 8192, [2048], F32) for i in range(2)]
    load_fm(lambda tt: mem_d[tt * 128:(tt + 1) * 128, :], MX_, 2, MSTG)
    rmsnorm(MX_, 0, MN_, 0, 64, 256, HB_OFF, blk=256)
    for cb in range(0, 8, 2):
        wres, (wv,) = wload([wsrc(wkv_d, 0, 16, cb * 128, 256)])
        for ci in range(2):
            p = psum()
            S.mm(p.ap[:, 0:256], [(wv[:, k, ci * 128:(ci + 1) * 128], MN_.ap[:, k, :]) for k in range(16)],
                 [wres, MN_.res], [p.res])
            act(MKT.ap[:, cb + ci, :], p.ap[:, 0:256], AF.Copy, [p.res], [MKT.res])
    for cb2 in range(2):
        banks = [psum() for _ in range(2)]
        for kh in range(2):
            wres, (wv,) = wload([wsrc(wkv_d, kh * 1024, 8, 1024 + cb2 * 512, 512)])
            for mt in range(2):
                p = banks[mt]
                S.mm(p.ap, [(MN_.ap[:, kh * 8 + k, mt * 128:(mt + 1) * 128], wv[:, k, :]) for k in range(8)],
                     [wres, MN_.res], [p.res], start=(kh == 0), stop=(kh == 1))
                if kh == 1:
                    S.op("dve", [p.res], [MV.res], lambda e: e.tensor_copy(MV.ap[:, mt, cb2 * 512:(cb2 + 1) * 512], p.ap))

    def ffn(wg_d, wu_d, wd_d, gcol):
        S.phase('ffn_norm')
        rmsnorm(X, 0, HB, 0, gcol, 1024, R_OFF)
        for g in range(4):
            S.phase(f'ffn_gu{g}')
            for f0 in range(0, 11, 2):
                nf = min(2, 11 - f0)
                c0 = (g * 11 + f0) * 128
                wres, (wgv,) = wload([wsrc(wg_d, 0, 16, c0, nf * 128)])
                for fi in range(nf):
                    for tb in range(2):
                        p = psum()
                        S.mm(p.ap, [(wgv[:, k, fi * 128:(fi + 1) * 128], HB.ap[:, k, tb * 512:(tb + 1) * 512])
                                    for k in range(16)], [wres, HB.res], [p.res])
                        sg = SGT[fi * 2 + tb]
                        act(sg.ap, p.ap, AF.Silu, [p.res], [sg.res])
                wres, (wuv,) = wload([wsrc(wu_d, 0, 16, c0, nf * 128)])
                for fi in range(nf):
                    f = f0 + fi
                    for tb in range(2):
                        p = psum()
                        S.mm(p.ap, [(wuv[:, k, fi * 128:(fi + 1) * 128], HB.ap[:, k, tb * 512:(tb + 1) * 512])
                                    for k in range(16)], [wres, HB.res], [p.res])
                        sg = SGT[fi * 2 + tb]
                        S.op("dve", [sg.res, p.res], [H1.res],
                             lambda e: e.tensor_tensor(H1.ap[:, f, tb * 512:(tb + 1) * 512], sg.ap, p.ap, ALU.mult))
            S.phase(f'ffn_dn{g}')
            for db in range(4):
                banks = [psum() for _ in range(8)]
                for (k0, nk) in ((0, 6), (6, 5)):
                    wres, (wdv,) = wload([wsrc(wd_d, (g * 11 + k0) * 128, nk, db * 512, 512)])
                    for dc in range(4):
                        for tb in range(2):
                            p = banks[dc * 2 + tb]
                            S.mm(p.ap, [(wdv[:, k, dc * 128:(dc + 1) * 128], H1.ap[:, k0 + k, tb * 512:(tb + 1) * 512])
                                        for k in range(nk)], [wres, H1.res], [p.res], start=(k0 == 0), stop=(k0 == 6))
                            if k0 == 6:
                                xv = X.ap[:, db * 4 + dc, tb * 512:(tb + 1) * 512]
                                S.op("dve", [p.res, X.res], [X.res],
                                     lambda e: e.scalar_tensor_tensor(xv, p.ap, 0.5, xv, ALU.mult, ALU.add))

    def fm_proj(col0, nchunks, evac):
        for cb in range(0, nchunks, 2):
            n = min(2, nchunks - cb)
            wres, (wv,) = wload([wsrc(win_d, 0, 16, col0 + cb * 128, n * 128)])
            for ci in range(n):
                p = psum()
                S.mm(p.ap, [(wv[:, k, ci * 128:(ci + 1) * 128], HBS.ap[:, k, :]) for k in range(16)],
                     [wres, HBS.res], [p.res])
                evac(cb + ci, p)

    def fm_proj_gen(col0, nchunks, evac):
        for cb in range(0, nchunks, 2):
            n = min(2, nchunks - cb)
            wres, (wv,) = wload([wsrc(win_d, 0, 16, col0 + cb * 128, n * 128)])
            for ci in range(n):
                p = psum()
                S.mm(p.ap, [(wv[:, k, ci * 128:(ci + 1) * 128], HBS.ap[:, k, :]) for k in range(16)],
                     [wres, HBS.res], [p.res])
                evac(cb + ci, p)
                yield

    def tm_proj(col0, evac):
        banks = [psum() for _ in range(4)]
        for kh in range(2):
            wres, (wv,) = wload([wsrc(win_d, kh * 1024, 8, col0, 512)])
            for tt in range(4):
                p = banks[tt]
                S.mm(p.ap, [(HBS.ap[:, kh * 8 + k, tt * 128:(tt + 1) * 128], wv[:, k, :]) for k in range(8)],
                     [wres, HBS.res], [p.res], start=(kh == 0), stop=(kh == 1))
                if kh == 1:
                    evac(tt, p)

    def mixer(t0):
        S.phase('mix_norm')
        rmsnorm(X, t0, HBS, 0, 16, 512, S2)
        S.phase('gla_glr_z')
        p = psum()
        S.mm(p.ap[0:16, :], [(WGLR.ap[:, k, :], HBS.ap[:, k, :]) for k in range(16)], [WGLR.res, HBS.res], [p.res])
        act(GLR.ap[0:16, :], p.ap[0:16, :], AF.Copy, [p.res], [GLR.res])
        for tt in range(4):
            p = psum()
            S.mm(p.ap, [(GLR.ap[0:17, tt * 128:(tt + 1) * 128], WGU.ap[0:17, :])], [GLR.res, WGU.res], [p.res])
            act(LA.ap[:, tt, :], p.ap, AF.Exp, [p.res], [LA.res], scale=-1.0)
            act(LA.ap[:, tt, :], LA.ap[:, tt, :], AF.Ln, [LA.res], [LA.res], bias=1.0)
        S.phase('gla_qk')
        wq_res0, (wq0,) = wload([wsrc(win_d, 0, 8, C_Q, 512)])
        wq_res1, (wq1,) = wload([wsrc(win_d, 1024, 8, C_Q, 512)])

        def cumT(h):
            pb = psum()
            for c in range(4):
                S.mm(pb.ap[:, c * 128:(c + 1) * 128], [(LA.ap[:, c, h * 128:(h + 1) * 128], MASKU.ap)],
                     [LA.res, MASKU.res], [pb.res], signal=(c == 3))
            return pb

        for h in range(4):
            pb = cumT(h)
            eb = EBT[h % 2]
            act(eb.ap, pb.ap, AF.Exp, [pb.res], [eb.res], scale=-1.0 / 16)
            S.op("dve", [eb.res], [EBL.res], lambda e: e.tensor_copy(EBL.ap[:, h:16:4], eb.ap[:, 127:512:128]))
            pq = psum()
            hs = slice(h * 128, (h + 1) * 128)
            S.mm(pq.ap, [(wq0[:, k, hs], HBS.ap[:, k, :]) for k in range(8)] + [(wq1[:, k, hs], HBS.ap[:, 8 + k, :]) for k in range(8)],
                 [wq_res0, wq_res1, HBS.res], [pq.res])
            S.op("dve", [pq.res, eb.res], [QT.res],
                 lambda e: e.scalar_tensor_tensor(QT.ap[:, h, :], pq.ap, 128 ** -0.5, eb.ap, ALU.mult, ALU.mult))
        wk_res0, (wk0,) = wload([wsrc(win_d, 0, 8, C_K, 512)])
        wk_res1, (wk1,) = wload([wsrc(win_d, 1024, 8, C_K, 512)])
        for h in range(4):
            pb = cumT(h)
            enb = ENBT[h % 2]
            act(enb.ap, pb.ap, AF.Exp, [pb.res], [enb.res], scale=1.0 / 16)
            pk = psum()
            hs = slice(h * 128, (h + 1) * 128)
            S.mm(pk.ap, [(wk0[:, k, hs], HBS.ap[:, k, :]) for k in range(8)] + [(wk1[:, k, hs], HBS.ap[:, 8 + k, :]) for k in range(8)],
                 [wk_res0, wk_res1, HBS.res], [pk.res])
            S.op("dve", [pk.res, enb.res], [KT.res], lambda e: e.tensor_tensor(KT.ap[:, h, :], pk.ap, enb.ap, ALU.mult))
        S.phase('gla_ktm')
        for c in range(4):
            pbt = psum()
            S.mm(pbt.ap, [(MASKU.ap, LA.ap[:, c, :])], [LA.res, MASKU.res], [pbt.res])
            act(ENBTM.ap, pbt.ap, AF.Exp, [pbt.res], [ENBTM.res], scale=1.0 / 16)
            pk = psum()
            csl_ = slice(c * 128, (c + 1) * 128)
            S.mm(pk.ap, [(HBS.ap[:, k, csl_], wk0[:, k, :]) for k in range(8)] + [(HBS.ap[:, 8 + k, csl_], wk1[:, k, :]) for k in range(8)],
                 [wk_res0, wk_res1, HBS.res], [pk.res])
            S.op("dve", [pk.res, ENBTM.res], [KTM.res], lambda e: e.tensor_tensor(KTM.ap[:, c, :], pk.ap, ENBTM.ap, ALU.mult))
        S.phase('gla_v')
        for cb2 in range(2):
            tm_proj(C_V + cb2 * 512,
                    lambda tt, p: act(VTM.ap[:, tt, cb2 * 512:(cb2 + 1) * 512], p.ap, AF.Copy, [p.res], [VTM.res]))
        S.phase('gla_r')

        def r_evac(ch, p):
            rt = RTMP[ch % 2]
            act(rt.ap, p.ap, AF.Silu, [p.res], [rt.res])
            S.op("dve", [rt.res, GON.res], [YGLA.res],
                 lambda e: e.tensor_scalar(YGLA.ap[:, ch, :], rt.ap, GON.ap[:, ch:ch + 1], None, ALU.mult))

        fm_proj(C_R, 8, r_evac)
        S.phase('gla_loop')
        su_gen = fm_proj_gen(C_SU, 8, lambda ch, p: act(YSG.ap[:, ch, :], p.ap, AF.Gelu, [p.res], [YSG.res]))

        def scores(c):
            csl = slice(c * 128, (c + 1) * 128)
            pa = psum()
            for h in range(4):
                S.mm(pa.ap[:, h * 128:(h + 1) * 128], [(KT.ap[:, h, csl], QT.ap[:, h, csl])], [KT.res, QT.res], [pa.res],
                     signal=(h == 3))
            pt = PT4[c % 2]
            S.op("dve", [pa.res, MASK4.res], [pt.res],
                 lambda e: e.tensor_tensor(pt.ap, pa.ap.rearrange("p (h t) -> p h t", t=128), MASK4.ap, ALU.mult))

        scores(0)
        for c in range(4):
            csl = slice(c * 128, (c + 1) * 128)
            pt = PT4[c % 2]
            pB = [psum(), psum()]
            for j in range(2):
                for h in range(4):
                    S.mm(pB[j].ap[:, h * 128:(h + 1) * 128],
                         [(VTM.ap[:, c, h * 256 + j * 128:h * 256 + (j + 1) * 128], pt.ap[:, h, :]),
                          (STB4.ap[:, h, j * 128:(j + 1) * 128], QT.ap[:, h, csl])],
                         [VTM.res, pt.res, STB4.res, QT.res], [pB[j].res], signal=(h == 3))
            pKV = [psum(), psum()]
            for h in range(4):
                S.mm(pKV[h // 2].ap[:, (h % 2) * 256:(h % 2 + 1) * 256],
                     [(KTM.ap[:, c, h * 128:(h + 1) * 128], VTM.ap[:, c, h * 256:(h + 1) * 256])],
                     [KTM.res, VTM.res], [pKV[h // 2].res], signal=(h % 2 == 1))
            if c < 3:
                scores(c + 1)
            for _ in range(2):
                next(su_gen, None)
            for b in range(2):
                sv_ = ST4.ap[:, 2 * b:2 * b + 2, :]
                S.op("dve", [pKV[b].res, ST4.res], [ST4.res],
                     lambda e: e.tensor_tensor(sv_, pKV[b].ap.rearrange("p (h e) -> p h e", e=256), sv_, ALU.add))
            eblb = EBL.ap[:, c * 4:(c + 1) * 4].unsqueeze(2).to_broadcast([128, 4, 256])
            S.op("dve", [ST4.res, EBL.res], [ST4.res], lambda e: e.tensor_tensor(ST4.ap, ST4.ap, eblb, ALU.mult))
            act(STB4.ap, ST4.ap, AF.Copy, [ST4.res], [STB4.res])
            for j in range(2):
                act(OSQ8.ap[:, j * 4:(j + 1) * 4, :], pB[j].ap.rearrange("p (h t) -> p h t", t=128), AF.Square,
                    [pB[j].res], [OSQ8.res])
            pss = psum()
            for h in range(4):
                S.mm(pss.ap[:, h * 128:(h + 1) * 128], [(ONESB.ap, OSQ8.ap[:, h, :]), (ONESB.ap, OSQ8.ap[:, 4 + h, :])],
                     [ONESB.res, OSQ8.res], [pss.res], signal=(h == 3))
            rsqrt_from_psum(RS4.ap, pss.ap.rearrange("p (h t) -> p h t", t=128), 1.0 / 256, pss.res, RS4.res)
            for j in range(2):
                S.op("dve", [pB[j].res, RS4.res], [OTT.res],
                     lambda e: e.tensor_tensor(OTT.ap[:, j * 4:(j + 1) * 4, :], pB[j].ap.rearrange("p (h t) -> p h t", t=128),
                                               RS4.ap, ALU.mult))
                yv = YGLA.ap[:, j:8:2, csl]
                S.op("dve", [OTT.res, YGLA.res], [YGLA.res],
                     lambda e: e.tensor_tensor(yv, OTT.ap[:, j * 4:(j + 1) * 4, :], yv, ALU.mult))
        S.phase('sg_u')
        for _ in su_gen:
            pass
        S.phase('sg_v')
        xq_gen = fm_proj_gen(C_XQ, 8, lambda ch, p: act(XQT.ap[:, ch, :], p.ap, AF.Copy, [p.res], [XQT.res], scale=1.0 / 16))
        for cb2 in range(2):
            lsl = slice(cb2 * 512, (cb2 + 1) * 512)
            tm_proj(C_SV + cb2 * 512, lambda tt, p: act(GV4.ap[:, tt, :], p.ap, AF.Gelu, [p.res], [GV4.res]))
            for _ in range(4):
                next(xq_gen, None)
            for tt in range(4):
                for gi in range(2):
                    S.op("dve", [GV4.res], [BNS8.res],
                         lambda e: e.bn_stats(BNS8.ap[:, tt * 2 + gi, :], GV4.ap[:, tt, gi * 256:(gi + 1) * 256]))
            for q8 in range(8):
                S.op("dve", [BNS8.res], [BNA8.res], lambda e: e.bn_aggr(BNA8.ap[:, q8, :], BNS8.ap[:, q8, :]))
            act(LNR8.ap, BNA8.ap[:, :, 1], AF.Ln, [BNA8.res], [LNR8.res], bias=EPS)
            act(LNR8.ap, LNR8.ap, AF.Exp, [LNR8.res], [LNR8.res], scale=-0.5)
            for tt in range(4):
                for gi in range(2):
                    gsl = slice(gi * 256, (gi + 1) * 256)
                    q8 = tt * 2 + gi
                    S.op("dve", [GV4.res, BNA8.res, LNR8.res], [GV4.res],
                         lambda e: e.tensor_scalar(GV4.ap[:, tt, gsl], GV4.ap[:, tt, gsl], BNA8.ap[:, q8, 0:1], LNR8.ap[:, q8:q8 + 1],
                                                   ALU.subtract, ALU.mult))
            lg = LNBC.ap[:, lsl].unsqueeze(1).to_broadcast([128, 4, 512])
            lb = LNBC.ap[:, 1024 + cb2 * 512:1024 + (cb2 + 1) * 512].unsqueeze(1).to_broadcast([128, 4, 512])
            S.op("dve", [GV4.res, LNBC.res], [GV4.res], lambda e: e.tensor_tensor(GV4.ap, GV4.ap, lg, ALU.mult))
            S.op("dve", [GV4.res, LNBC.res], [SVTM.res], lambda e: e.tensor_tensor(SVTM.ap[:, :, lsl], GV4.ap, lb, ALU.add))
        S.phase('sg_mix')
        for g in range(4):
            for cj in range(2):
                p = psum()
                for tt in range(4):
                    S.mm(p.ap[:, tt * 128:(tt + 1) * 128],
                         [(SVTM.ap[:, tt, g * 256 + cj * 128:g * 256 + (cj + 1) * 128], WST.ap[:, g, :]),
                          (ONESF.ap[0:1, :], BSROW.ap[0:1, g * 128:(g + 1) * 128])],
                         [SVTM.res, WST.res, ONESF.res, BSROW.res], [p.res], signal=(tt == 3))
                yv = YSG.ap[:, g * 2 + cj, :]
                S.op("dve", [p.res, YSG.res], [YSG.res], lambda e: e.tensor_tensor(yv, p.ap, yv, ALU.mult))
        S.phase('xa_q')
        for _ in xq_gen:
            pass
        S.phase('xa_attn')
        for tt in range(4):
            tsl = slice(tt * 128, (tt + 1) * 128)
            pf, pt2 = PF[tt % 2], PT2[tt % 2]
            psc = [psum(), psum()]
            for h in range(4):
                S.mm(psc[h // 2].ap[:, (h % 2) * 256:(h % 2 + 1) * 256],
                     [(XQT.ap[:, h * 2 + j, tsl], MKT.ap[:, h * 2 + j, :]) for j in range(2)],
                     [XQT.res, MKT.res], [psc[h // 2].res], signal=(h % 2 == 1))
            for b in range(2):
                S.op("dve", [psc[b].res], [MXB.res],
                     lambda e: e.tensor_reduce(MXB.ap[:, 2 * b:2 * b + 2], psc[b].ap.rearrange("p (h m) -> p h m", m=256), AX.X, ALU.max))
            S.op("dve", [MXB.res], [NMX.res], lambda e: e.tensor_scalar(NMX.ap, MXB.ap, -1.0, None, ALU.mult))
            for h in range(4):
                act(pf.ap[:, h, :], psc[h // 2].ap[:, (h % 2) * 256:(h % 2 + 1) * 256], AF.Exp,
                    [psc[h // 2].res, NMX.res], [pf.res, RSUM.res], bias=NMX.ap[:, h:h + 1], accum_out=RSUM.ap[:, h:h + 1])
            S.op("dve", [RSUM.res], [RINV.res], lambda e: e.reciprocal(RINV.ap, RSUM.ap))
            for h in range(4):
                S.op("dve", [pf.res, RINV.res], [pf.res],
                     lambda e: e.tensor_scalar(pf.ap[:, h, :], pf.ap[:, h, :], RINV.ap[:, h:h + 1], None, ALU.mult))
            for b in range(2):
                ptp = psum()
                for q in range(4):
                    h, mt = b * 2 + q // 2, q % 2
                    S.tr(ptp.ap[:, q * 128:(q + 1) * 128], pf.ap[:, h, mt * 128:(mt + 1) * 128], IDENT.ap,
                         [pf.res, IDENT.res], [ptp.res], signal=(q == 3))
                ov = pt2.ap[:, b * 4:(b + 1) * 4, :]
                iv = ptp.ap.rearrange("p (a b) -> p a b", b=128)
                if b == 0:
                    act(ov, iv, AF.Copy, [ptp.res], [pt2.res])
                else:
                    S.op("dve", [ptp.res], [pt2.res], lambda e: e.tensor_copy(ov, iv))
            for b in range(2):
                po = psum()
                for q in range(4):
                    hj = b * 4 + q
                    h, j = hj // 2, hj % 2
                    S.mm(po.ap[:, q * 128:(q + 1) * 128],
                         [(MV.ap[:, mt, h * 256 + j * 128:h * 256 + (j + 1) * 128], pt2.ap[:, h * 2 + mt, :]) for mt in range(2)],
                         [MV.res, pt2.res], [po.res], signal=(q == 3))
                ov = YXA.ap[:, b * 4:(b + 1) * 4, tsl]
                iv = po.ap.rearrange("p (a b) -> p a b", b=128)
                if b == 0:
                    act(ov, iv, AF.Copy, [po.res], [YXA.res])
                else:
                    S.op("dve", [po.res], [YXA.res], lambda e: e.tensor_copy(ov, iv))
        ys = [YGLA, YSG, YXA]
        S.phase('merge')
        for jg in range(4):
            mrg = MRG[jg % 2]
            for i in range(3):
                gt = GT[i % 2]
                gbanks = [psum() for _ in range(4)]
                for kh in range(2):
                    wres, (wv,) = wload([wsrc(win_d, kh * 1024, 8, C_GATE + i * 2048 + jg * 512, 512)])
                    for ji in range(4):
                        p = gbanks[ji]
                        S.mm(p.ap, [(wv[:, k, ji * 128:(ji + 1) * 128], HBS.ap[:, kh * 8 + k, :]) for k in range(8)],
                             [wres, HBS.res], [p.res], start=(kh == 0), stop=(kh == 1))
                        if kh == 1:
                            act(gt.ap[:, ji, :], p.ap, AF.Sigmoid, [p.res], [gt.res])
                wres, (wv,) = wload([wsrc(wbr_d, i * 1024, 8, jg * 512, 512)])
                for ji in range(4):
                    p = psum()
                    S.mm(p.ap, [(wv[:, k, ji * 128:(ji + 1) * 128], ys[i].ap[:, k, :]) for k in range(8)], [wres, ys[i].res], [p.res])
                    if i == 0:
                        S.op("dve", [p.res, gt.res], [ACC.res], lambda e: e.tensor_tensor(ACC.ap[:, ji, :], gt.ap[:, ji, :], p.ap, ALU.mult))
                    else:
                        tt_ = TT_[ji % 2]
                        S.op("dve", [p.res, gt.res], [tt_.res], lambda e: e.tensor_tensor(tt_.ap, gt.ap[:, ji, :], p.ap, ALU.mult))
                        if i == 1:
                            S.op("dve", [tt_.res, ACC.res], [ACC.res], lambda e: e.tensor_tensor(ACC.ap[:, ji, :], ACC.ap[:, ji, :], tt_.ap, ALU.add))
                        else:
                            S.op("dve", [tt_.res, ACC.res], [mrg.res], lambda e: e.tensor_tensor(mrg.ap[:, ji, :], ACC.ap[:, ji, :], tt_.ap, ALU.add))
            for oh in range(2):
                wres, (wv,) = wload([wsrc(wout_d, jg * 512, 4, oh * 1024, 1024)])
                for d8 in range(8):
                    dc = oh * 8 + d8
                    p = psum()
                    S.mm(p.ap, [(wv[:, k, d8 * 128:(d8 + 1) * 128], mrg.ap[:, k, :]) for k in range(4)], [wres, mrg.res], [p.res])
                    xv = X.ap[:, dc, t0:t0 + 512]
                    S.op("dve", [p.res, X.res], [X.res], lambda e: e.tensor_tensor(xv, p.ap, xv, ALU.add))

    for tile in range(2):
        row0 = tile * 1024
        S.phase('load_x')
        load_fm(lambda tt: x_d[row0 + tt * 128: row0 + (tt + 1) * 128, :], X, 8, STG)
        ffn(*ffw[0], 0)
        for sub in range(2):
            mixer(sub * 512)
        ffn(*ffw[1], 32)
        S.phase('final_norm')
        rmsnorm(X, 0, X, 0, 48, 1024, E_OFF)
        S.phase('store')
        store_fm(X, row0, 8, STG)
    for k in ("out0", "out1"):
        nc.sync.wait_ge(S.sem[k], S.cnt[k])
    S.phase('end')
    nc._phases = S.phases
    return nc


_NC_CACHE = {}


def _fm(v):
    return np.ascontiguousarray(np.asarray(v, np.float32).reshape(16, 128).T)


def kernel(x, mem, ffn1_norm, ffn1_w_gate, ffn1_w_up, ffn1_w_down, mix_norm, mem_norm,
           w_in, gla_w_gate_up, gla_gate_bias, gla_out_norm, sg_ln_g, sg_ln_b, sg_w_s, sg_b_s,
           w_kv_mem, w_branch, w_out, ffn2_norm, ffn2_w_gate, ffn2_w_up, ffn2_w_down, final_norm):
    f = lambda a: np.ascontiguousarray(np.asarray(a, np.float32))
    x = f(x)
    mem = f(mem)
    B = x.shape[0]
    gains = np.concatenate([_fm(f(ffn1_norm)[0]), _fm(f(mix_norm)[0]), _fm(f(ffn2_norm)[0]),
                            _fm(f(final_norm)), _fm(f(mem_norm)[0])], axis=1)
    gon = np.ascontiguousarray(f(gla_out_norm)[0].reshape(8, 128).T)
    lnbc = np.ascontiguousarray(np.broadcast_to(
        np.concatenate([f(sg_ln_g)[0].reshape(-1), f(sg_ln_b)[0].reshape(-1)])[None, :], (128, 2048)))
    wst = np.ascontiguousarray(f(sg_w_s)[0].transpose(2, 0, 1).reshape(128, 512))
    wgu = np.ascontiguousarray(np.concatenate([f(gla_w_gate_up)[0], f(gla_gate_bias)[0][None, :]], axis=0))
    bs = np.ascontiguousarray(f(sg_b_s)[0].reshape(1, 512))
    cst = np.ascontiguousarray(np.concatenate([np.eye(128, dtype=np.float32),
                                               np.triu(np.ones((128, 128), np.float32))], axis=1))
    shared = {
        "w_in": f(w_in)[0], "f1g": f(ffn1_w_gate)[0], "f1u": f(ffn1_w_up)[0], "f1d": f(ffn1_w_down)[0],
        "f2g": f(ffn2_w_gate)[0], "f2u": f(ffn2_w_up)[0], "f2d": f(ffn2_w_down)[0],
        "w_kv": f(w_kv_mem)[0], "w_br": f(w_branch)[0].reshape(3072, 2048), "w_out": f(w_out)[0],
        "gains": gains, "gon": gon, "lnbc": lnbc, "wst": wst, "wgu": wgu, "bs": bs, "cst": cst,
    }
    if "nc" not in _NC_CACHE:
        _NC_CACHE["nc"] = build_nc()
    nc = _NC_CACHE["nc"]
    in_maps = []
    for b in range(B):
        m = dict(shared)
        m["x"] = x[b]
        m["mem"] = mem[b]
        in_maps.append(m)
    res = run_bass_kernel_spmd(nc, in_maps, core_ids=list(range(B)))
    return np.stack([np.asarray(r["y"], np.float32) for r in res.results], axis=0)
```

```python
import numpy as np
import concourse.bass as bass
import concourse.mybir as mybir
from concourse.bass_utils import run_bass_kernel_spmd

F32 = mybir.dt.float32
BF16 = mybir.dt.bfloat16
AF = mybir.ActivationFunctionType
ALU = mybir.AluOpType
AX = mybir.AxisListType

D = 2048
SEQ = 2048
DFF = 5632
EPS = 1e-6
C_Q, C_K, C_V, C_R, C_GLR, C_SU, C_SV, C_XQ, C_GATE = 0, 512, 1024, 2048, 3072, 3088, 4112, 5136, 6160


class Res:
    __slots__ = ("name", "lo", "hi", "w", "r", "al")

    def __init__(self, name, lo=None, hi=None):
        self.name, self.lo, self.hi = name, lo, hi
        self.w = None
        self.r = {}
        self.al = []


class Buf:
    __slots__ = ("ap", "res")

    def __init__(self, ap, res):
        self.ap, self.res = ap, res


class Sched:
    def __init__(self, nc):
        self.nc = nc
        self.eng = {"pe": nc.tensor, "act": nc.scalar, "dve": nc.vector, "pool": nc.gpsimd, "sp": nc.sync}
        self.sem = {}
        self.cnt = {}
        self.seen = {e: {} for e in self.eng}
        for e in self.eng:
            self.sem[e] = nc.alloc_semaphore("s_" + e)
            self.cnt[e] = 0
        self.all_res = []
        self.npe = 0
        self.phases = []

    def phase(self, name):
        self.phases.append((name, self.npe))

    def _sync(self, e, reads, writes):
        need = {}

        def add(ev):
            if ev is None:
                return
            k, v = ev
            if need.get(k, 0) < v:
                need[k] = v

        for r in reads:
            add(r.w)
            for t in r.al:
                add(t.w)
        for w in writes:
            for t in [w] + w.al:
                for k, v in t.r.items():
                    add((k, v))
                add(t.w)
        seen = self.seen[e]
        eng = self.eng[e]
        for k, v in need.items():
            if e == "pe" and k == "pe":
                continue
            if seen.get(k, 0) >= v:
                continue
            eng.wait_ge(self.sem[k], v)
            seen[k] = v

    def _post(self, key, val, reads, writes):
        for r in reads:
            r.r[key] = val
        for w in writes:
            w.w = (key, val)
            w.r = {}

    def op(self, e, reads, writes, fn):
        self._sync(e, reads, writes)
        ins = fn(self.eng[e])
        self.cnt[e] += 1
        ins.then_inc(self.sem[e], 1)
        self._post(e, self.cnt[e], reads, writes)

    def mm(self, out_ap, pairs, reads, writes, start=True, stop=True, signal=True):
        self._sync("pe", reads, writes)
        n = len(pairs)
        ins = None
        self.npe += n
        for i, (l, r) in enumerate(pairs):
            ins = self.nc.tensor.matmul(out_ap, lhsT=l, rhs=r, start=(start and i == 0), stop=(stop and i == n - 1))
        if signal:
            self.cnt["pe"] += 1
            ins.then_inc(self.sem["pe"], 1)
            self._post("pe", self.cnt["pe"], reads, writes)
        else:
            self._post("pe", self.cnt["pe"] + 1, reads, writes)

    def tr(self, out_ap, in_ap, ident_ap, reads, writes, signal=True):
        self._sync("pe", reads, writes)
        self.npe += 1
        ins = self.nc.tensor.transpose(out_ap, in_ap, ident_ap)
        if signal:
            self.cnt["pe"] += 1
            ins.then_inc(self.sem["pe"], 1)
            self._post("pe", self.cnt["pe"], reads, writes)
        else:
            self._post("pe", self.cnt["pe"] + 1, reads, writes)

    def dma(self, q, key, parts, reads, writes):
        if key not in self.sem:
            self.sem[key] = self.nc.alloc_semaphore("d_" + key)
            self.cnt[key] = 0
        self._sync(q, reads, writes)
        for (o, i) in parts:
            ins = self.eng[q].dma_start(out=o, in_=i)
            self.cnt[key] += 16
            ins.then_inc(self.sem[key], 16)
        self._post(key, self.cnt[key], reads, writes)


def build_nc():
    nc = bass.Bass("TRN2", target_bir_lowering=False)
    S = Sched(nc)

    def din(name, shape):
        return nc.dram_tensor(name, list(shape), F32, kind="ExternalInput").ap()

    x_d = din("x", [SEQ, D])
    mem_d = din("mem", [256, D])
    win_d = din("w_in", [D, 12304])
    ffw = []
    for i in (1, 2):
        ffw.append((din(f"f{i}g", [D, DFF]), din(f"f{i}u", [D, DFF]), din(f"f{i}d", [DFF, D])))
    wkv_d = din("w_kv", [D, D])
    wbr_d = din("w_br", [3072, D])
    wout_d = din("w_out", [D, D])
    gains_d = din("gains", [128, 80])
    gon_d = din("gon", [128, 8])
    lnbc_d = din("lnbc", [128, 2048])
    wst_d = din("wst", [128, 512])
    wgu_d = din("wgu", [17, 512])
    bs_d = din("bs", [1, 512])
    cst_d = din("cst", [128, 256])
    y_d = nc.dram_tensor("y", [SEQ, D], F32, kind="ExternalOutput").ap()

    TOTAL = 212480
    BIG = nc.alloc_sbuf_tensor("BIG", [128, TOTAL // 2], BF16)

    def carve(name, off, shape, dtype):
        esz = 2 if dtype == BF16 else 4
        n = 1
        for s_ in shape:
            n *= s_
        assert off % 4 == 0 and off + n * esz <= TOTAL, (name, off, n * esz)
        ap = BIG[:, off // 2: off // 2 + n * esz // 2]
        if dtype == F32:
            ap = ap.bitcast(F32)
        if len(shape) == 2:
            ap = ap.rearrange("p (a b) -> p a b", b=shape[1])
        res = Res(name, off, off + n * esz)
        for o in S.all_res:
            if o.lo < res.hi and res.lo < o.hi:
                o.al.append(res)
                res.al.append(o)
        S.all_res.append(res)
        return Buf(ap, res)

    X_OFF = 0
    HB_OFF = 65536
    W_OFF = 98304
    R_OFF = 131072
    E_OFF = 153600
    BS_OFF = 180224
    C_OFF = 182272
    MKT_OFF = 196608
    MV_OFF = 200704
    ST_OFF = 204800
    STB_OFF = 208896
    SM_OFF = 210944
    S1 = R_OFF
    S2 = HB_OFF + 16384

    X = carve("X", X_OFF, [16, 1024], F32)
    HB = carve("HB", HB_OFF, [16, 1024], BF16)
    HBS = carve("HBS", HB_OFF, [16, 512], BF16)
    WS = [carve(f"W{i}", W_OFF + i * 8192, [4096], BF16) for i in range(4)]
    H1 = carve("H1", R_OFF, [11, 1024], BF16)
    STG = [carve(f"STG{i}", R_OFF + i * 8192, [2048], F32) for i in range(2)]
    SGT = [carve(f"SGT{i}", E_OFF + i * 1024, [512], BF16) for i in range(4)]
    o = C_OFF
    GAINS = carve("GAINS", o, [80], F32); o += 320
    GON = carve("GON", o, [8], F32); o += 32
    IDENT = carve("IDENT", o, [128], F32); o += 512
    MASKU = carve("MASKU", o, [128], F32); o += 512
    ONESB = carve("ONESB", o, [128], BF16); o += 256
    ONESF = carve("ONESF", o, [128], F32); o += 512
    WGU = carve("WGU", o, [512], F32); o += 2048
    WST = carve("WST", o, [4, 128], BF16); o += 1024
    WGLR = carve("WGLR", o, [16, 16], BF16); o += 512
    LNBC = carve("LNBC", o, [2048], F32); o += 8192
    assert o <= C_OFF + 14336
    BSROW = carve("BSROW", BS_OFF, [512], F32)
    MKT = carve("MKT", MKT_OFF, [8, 256], BF16)
    MV = carve("MV", MV_OFF, [2, 1024], BF16)
    ST = [carve(f"ST{h}", ST_OFF + h * 1024, [256], F32) for h in range(4)]
    STB = [carve(f"STB{h}", STB_OFF + h * 512, [256], BF16) for h in range(4)]
    o = SM_OFF
    EBL = carve("EBL", o, [16], F32); o += 64
    BNS = carve("BNS", o, [2, 6], F32); o += 48
    BNA = carve("BNA", o, [2, 2], F32); o += 16
    LNR = carve("LNR", o, [2], F32); o += 8
    o += 8
    MXB = carve("MXB", o, [4], F32); o += 16
    NMX = carve("NMX", o, [4], F32); o += 16
    RSUM = carve("RSUM", o, [4], F32); o += 16
    RINV = carve("RINV", o, [4], F32); o += 16
    assert o <= TOTAL
    QT = carve("QT", S1 + 0, [4, 512], BF16)
    KT = carve("KT", S1 + 4096, [4, 512], BF16)
    KTM = carve("KTM", S1 + 8192, [4, 512], BF16)
    VTM = carve("VTM", S1 + 12288, [4, 1024], BF16)
    GLR = carve("GLR", S1 + 20480, [512], F32)
    LA = carve("LA", S2 + 0, [4, 512], F32)
    EBT = [carve(f"EBT{i}", S2 + 8192 + i * 2048, [512], F32) for i in range(2)]
    ENBT = [carve(f"ENBT{i}", S2 + 12288 + i * 2048, [512], F32) for i in range(2)]
    YGLA = carve("YGLA", E_OFF, [8, 512], BF16)
    YSG = carve("YSG", E_OFF + 8192, [8, 512], BF16)
    YXA = carve("YXA", E_OFF + 16384, [8, 512], BF16)
    ENBTM = carve("ENBTM", E_OFF + 24576, [512], F32)
    PT4 = [carve(f"PT4_{i}", S2 + i * 1024, [4, 128], BF16) for i in range(2)]
    OSQ8 = carve("OSQ8", S2 + 2048, [8, 128], BF16)
    RS4 = carve("RS4", S2 + 4096, [4, 128], F32)
    OTT = carve("OTT", S2 + 6144, [8, 128], F32)
    RTMP = [carve(f"RTMP{i}", S2 + 10240 + i * 2048, [512], F32) for i in range(2)]
    MASK4 = carve("MASK4", SM_OFF + 256, [4, 128], BF16)
    ST4 = carve("ST4", ST_OFF, [4, 256], F32)
    STB4 = carve("STB4", STB_OFF, [4, 256], BF16)
    GV4 = carve("GV4", S2, [4, 512], F32)
    BNS8 = carve("BNS8", SM_OFF + 1280, [8, 6], F32)
    BNA8 = carve("BNA8", SM_OFF + 1280 + 192, [8, 2], F32)
    LNR8 = carve("LNR8", SM_OFF + 208, [8], F32)
    SVTM = carve("SVTM", S1 + 0, [4, 1024], BF16)
    XQT = carve("XQT", S1 + 8192, [8, 512], BF16)
    PF = [carve(f"PF{i}", S2 + 8192 + i * 4096, [4, 256], F32) for i in range(2)]
    PT2 = [carve(f"PT2{i}", S1 + 16384 + i * 2048, [8, 128], BF16) for i in range(2)]
    ACC = carve("ACC", S2 + 0, [4, 512], F32)
    GT = [carve(f"GT{i}", S2 + 8192 + i * 4096, [4, 512], BF16) for i in range(2)]
    MRG = [carve(f"MRG{i}", S1 + i * 4096, [4, 512], BF16) for i in range(2)]
    TT_ = [carve(f"TT{i}", S1 + 8192 + i * 2048, [512], F32) for i in range(2)]

    PS = []
    for i in range(8):
        t = nc.alloc_psum_tensor(f"ps{i}", [128, 512], F32)
        PS.append(Buf(t[:, :], Res(f"ps{i}")))
    state = {"pi": 0, "wi": 0}

    def psum():
        b = PS[state["pi"] % 8]
        state["pi"] += 1
        return b

    def wsrc(d2, r0, nk, c0, ncol):
        return d2[r0:r0 + nk * 128, c0:c0 + ncol].rearrange("(k p) c -> p k c", p=128)

    def wload(srcs):
        idx = state["wi"] % 4
        state["wi"] += 1
        slot = WS[idx]
        off = 0
        views, parts = [], []
        for s_ in srcs:
            nk, ncol = s_.shape[1], s_.shape[2]
            v = slot.ap[:, off:off + nk * ncol].rearrange("p (k c) -> p k c", c=ncol)
            parts.append((v, s_))
            views.append(v)
            off += nk * ncol
        assert off <= 4096
        S.dma("pool", f"w{idx}", parts, [], [slot.res])
        return slot.res, views

    def act(out, in_, func, reads, writes, **kw):
        S.op("act", reads, writes, lambda e: e.activation(out, in_, func, **kw))

    def rsqrt_from_psum(dst, ps_ap, scale, reads_ps, dres):
        act(dst, ps_ap, AF.Ln, [reads_ps], [dres], bias=EPS, scale=scale)
        act(dst, dst, AF.Exp, [dres], [dres], scale=-0.5)

    def rmsnorm(src, s_t0, dst, d_t0, gcol, ntok, tmp_off, blk=512):
        SQ = [carve(f"sq{tmp_off}_{i}", tmp_off + i * 1024, [512], BF16) for i in range(4)]
        RS = carve(f"rs{tmp_off}", tmp_off + 4096, [512], F32)
        for tb in range(ntok // blk):
            ssl = slice(s_t0 + tb * blk, s_t0 + (tb + 1) * blk)
            dsl = slice(d_t0 + tb * blk, d_t0 + (tb + 1) * blk)
            p = psum()
            for c in range(16):
                sq = SQ[c % 4]
                if c % 2 == 0:
                    act(sq.ap[:, 0:blk], src.ap[:, c, ssl], AF.Square, [src.res], [sq.res])
                else:
                    S.op("dve", [src.res], [sq.res],
                         lambda e: e.tensor_tensor(sq.ap[:, 0:blk], src.ap[:, c, ssl], src.ap[:, c, ssl], ALU.mult))
                S.mm(p.ap[:, 0:blk], [(ONESB.ap, sq.ap[:, 0:blk])], [sq.res, ONESB.res], [p.res],
                     start=(c == 0), stop=(c == 15))
            rsqrt_from_psum(RS.ap[:, 0:blk], p.ap[:, 0:blk], 1.0 / D, p.res, RS.res)
            for c in range(16):
                S.op("dve", [src.res, RS.res, GAINS.res], [dst.res],
                     lambda e: e.scalar_tensor_tensor(dst.ap[:, c, dsl], src.ap[:, c, ssl],
                                                      GAINS.ap[:, gcol + c:gcol + c + 1], RS.ap[:, 0:blk],
                                                      ALU.mult, ALU.mult))

    def load_fm(dram_rows, dst, ntt, stg):
        for tt in range(ntt):
            st = stg[tt % 2]
            S.dma("sp", f"stg{tt % 2}", [(st.ap, dram_rows(tt))], [], [st.res])
            for cb in range(4):
                p = psum()
                for ci in range(4):
                    c = cb * 4 + ci
                    S.tr(p.ap[:, ci * 128:(ci + 1) * 128], st.ap[:, c * 128:(c + 1) * 128], IDENT.ap,
                         [st.res, IDENT.res], [p.res], signal=(ci == 3))
                ov = dst.ap[:, cb * 4:cb * 4 + 4, tt * 128:(tt + 1) * 128]
                iv = p.ap.rearrange("p (a b) -> p a b", b=128)
                if cb % 2 == 0:
                    act(ov, iv, AF.Copy, [p.res], [dst.res])
                else:
                    S.op("dve", [p.res], [dst.res], lambda e: e.tensor_copy(ov, iv))

    def store_fm(src, row0, ntt, stg):
        for tt in range(ntt):
            st = stg[tt % 2]
            for cb in range(4):
                p = psum()
                for ci in range(4):
                    c = cb * 4 + ci
                    S.tr(p.ap[:, ci * 128:(ci + 1) * 128], src.ap[:, c, tt * 128:(tt + 1) * 128], IDENT.ap,
                         [src.res, IDENT.res], [p.res], signal=(ci == 3))
                ov = st.ap[:, cb * 512:(cb + 1) * 512]
                if cb % 2 == 0:
                    act(ov, p.ap, AF.Copy, [p.res], [st.res])
                else:
                    S.op("dve", [p.res], [st.res], lambda e: e.tensor_copy(ov, p.ap))
            S.dma("sp", f"out{tt % 2}", [(y_d[row0 + tt * 128: row0 + (tt + 1) * 128, :], st.ap)], [st.res], [])

    cparts = [
        (GAINS.ap, gains_d), (GON.ap, gon_d), (IDENT.ap, cst_d[:, 0:128]), (MASKU.ap, cst_d[:, 128:256]),
        (WGU.ap[0:17, :], wgu_d), (BSROW.ap[0:1, :], bs_d), (LNBC.ap, lnbc_d),
    ]
    S.dma("sp", "cst", cparts, [], [GAINS.res, GON.res, IDENT.res, MASKU.res, WGU.res, BSROW.res, LNBC.res])
    S.dma("pool", "wglr", [(WGLR.ap, wsrc(win_d, 0, 16, C_GLR, 16))], [], [WGLR.res])
    S.op("dve", [], [ONESB.res], lambda e: e.memset(ONESB.ap, 1.0))
    S.op("dve", [], [ONESF.res], lambda e: e.memset(ONESF.ap, 1.0))
    for h in range(4):
        S.op("dve", [], [ST[h].res], lambda e: e.memset(ST[h].ap, 0.0))
        S.op("dve", [], [STB[h].res], lambda e: e.memset(STB[h].ap, 0.0))
    S.op("dve", [], [GLR.res], lambda e: e.memset(GLR.ap[0:32, :], 1.0))
    WTMP = carve("WTMP", E_OFF, [512], F32)
    S.dma("sp", "cst2", [(WTMP.ap, wst_d)], [], [WTMP.res])
    for g in range(4):
        S.op("dve", [WTMP.res, MASKU.res], [WST.res],
             lambda e: e.tensor_tensor(WST.ap[:, g, :], WTMP.ap[:, g * 128:(g + 1) * 128], MASKU.ap, ALU.mult))

    for h in range(4):
        S.op("dve", [MASKU.res], [MASK4.res], lambda e: e.tensor_copy(MASK4.ap[:, h, :], MASKU.ap))
    S.phase('mem_init')
    MX_ = carve("MEMX", R_OFF, [16, 256], F32)
    MN_ = carve("MEMN", E_OFF + 16384, [16, 256], BF16)
    MSTG = [carve(f"MSTG{i}", E_OFF + i * 8192, [2048], F32) for i in range(2)]
    load_fm(lambda tt: mem_d[tt * 128:(tt + 1) * 128, :], MX_, 2, MSTG)
    rmsnorm(MX_, 0, MN_, 0, 64, 256, HB_OFF, blk=256)
    for cb in range(0, 8, 2):
        wres, (wv,) = wload([wsrc(wkv_d, 0, 16, cb * 128, 256)])
        for ci in range(2):
            p = psum()
            S.mm(p.ap[:, 0:256], [(wv[:, k, ci * 128:(ci + 1) * 128], MN_.ap[:, k, :]) for k in range(16)],
                 [wres, MN_.res], [p.res])
            act(MKT.ap[:, cb + ci, :], p.ap[:, 0:256], AF.Copy, [p.res], [MKT.res])
    for cb2 in range(2):
        banks = [psum() for _ in range(2)]
        for kh in range(2):
            wres, (wv,) = wload([wsrc(wkv_d, kh * 1024, 8, 1024 + cb2 * 512, 512)])
            for mt in range(2):
                p = banks[mt]
                S.mm(p.ap, [(MN_.ap[:, kh * 8 + k, mt * 128:(mt + 1) * 128], wv[:, k, :]) for k in range(8)],
                     [wres, MN_.res], [p.res], start=(kh == 0), stop=(kh == 1))
                if kh == 1:
                    S.op("dve", [p.res], [MV.res], lambda e: e.tensor_copy(MV.ap[:, mt, cb2 * 512:(cb2 + 1) * 512], p.ap))

    def ffn(wg_d, wu_d, wd_d, gcol):
        S.phase('ffn_norm')
        rmsnorm(X, 0, HB, 0, gcol, 1024, R_OFF)
        for g in range(4):
            S.phase(f'ffn_gu{g}')
            for f0 in range(0, 11, 2):
                nf = min(2, 11 - f0)
                c0 = (g * 11 + f0) * 128
                wres, (wgv,) = wload([wsrc(wg_d, 0, 16, c0, nf * 128)])
                for fi in range(nf):
                    for tb in range(2):
                        p = psum()
                        S.mm(p.ap, [(wgv[:, k, fi * 128:(fi + 1) * 128], HB.ap[:, k, tb * 512:(tb + 1) * 512])
                                    for k in range(16)], [wres, HB.res], [p.res])
                        sg = SGT[fi * 2 + tb]
                        act(sg.ap, p.ap, AF.Silu, [p.res], [sg.res])
                wres, (wuv,) = wload([wsrc(wu_d, 0, 16, c0, nf * 128)])
                for fi in range(nf):
                    f = f0 + fi
                    for tb in range(2):
                        p = psum()
                        S.mm(p.ap, [(wuv[:, k, fi * 128:(fi + 1) * 128], HB.ap[:, k, tb * 512:(tb + 1) * 512])
                                    for k in range(16)], [wres, HB.res], [p.res])
                        sg = SGT[fi * 2 + tb]
                        S.op("dve", [sg.res, p.res], [H1.res],
                             lambda e: e.tensor_tensor(H1.ap[:, f, tb * 512:(tb + 1) * 512], sg.ap, p.ap, ALU.mult))
            S.phase(f'ffn_dn{g}')
            for db in range(4):
                banks = [psum() for _ in range(8)]
                for (k0, nk) in ((0, 6), (6, 5)):
                    wres, (wdv,) = wload([wsrc(wd_d, (g * 11 + k0) * 128, nk, db * 512, 512)])
                    for dc in range(4):
                        for tb in range(2):
                            p = banks[dc * 2 + tb]
                            S.mm(p.ap, [(wdv[:, k, dc * 128:(dc + 1) * 128], H1.ap[:, k0 + k, tb * 512:(tb + 1) * 512])
                                        for k in range(nk)], [wres, H1.res], [p.res], start=(k0 == 0), stop=(k0 == 6))
                            if k0 == 6:
                                xv = X.ap[:, db * 4 + dc, tb * 512:(tb + 1) * 512]
                                S.op("dve", [p.res, X.res], [X.res],
                                     lambda e: e.scalar_tensor_tensor(xv, p.ap, 0.5, xv, ALU.mult, ALU.add))

    def fm_proj(col0, nchunks, evac):
        for cb in range(0, nchunks, 2):
            n = min(2, nchunks - cb)
            wres, (wv,) = wload([wsrc(win_d, 0, 16, col0 + cb * 128, n * 128)])
            for ci in range(n):
                p = psum()
                S.mm(p.ap, [(wv[:, k, ci * 128:(ci + 1) * 128], HBS.ap[:, k, :]) for k in range(16)],
                     [wres, HBS.res], [p.res])
                evac(cb + ci, p)

    def fm_proj_gen(col0, nchunks, evac):
        for cb in range(0, nchunks, 2):
            n = min(2, nchunks - cb)
            wres, (wv,) = wload([wsrc(win_d, 0, 16, col0 + cb * 128, n * 128)])
            for ci in range(n):
                p = psum()
                S.mm(p.ap, [(wv[:, k, ci * 128:(ci + 1) * 128], HBS.ap[:, k, :]) for k in range(16)],
                     [wres, HBS.res], [p.res])
                evac(cb + ci, p)
                yield

    def tm_proj(col0, evac):
        banks = [psum() for _ in range(4)]
        for kh in range(2):
            wres, (wv,) = wload([wsrc(win_d, kh * 1024, 8, col0, 512)])
            for tt in range(4):
                p = banks[tt]
                S.mm(p.ap, [(HBS.ap[:, kh * 8 + k, tt * 128:(tt + 1) * 128], wv[:, k, :]) for k in range(8)],
                     [wres, HBS.res], [p.res], start=(kh == 0), stop=(kh == 1))
                if kh == 1:
                    evac(tt, p)

    def mixer(t0):
        S.phase('mix_norm')
        rmsnorm(X, t0, HBS, 0, 16, 512, S2)
        S.phase('gla_glr_z')
        p = psum()
        S.mm(p.ap[0:16, :], [(WGLR.ap[:, k, :], HBS.ap[:, k, :]) for k in range(16)], [WGLR.res, HBS.res], [p.res])
        act(GLR.ap[0:16, :], p.ap[0:16, :], AF.Copy, [p.res], [GLR.res])
        for tt in range(4):
            p = psum()
            S.mm(p.ap, [(GLR.ap[0:17, tt * 128:(tt + 1) * 128], WGU.ap[0:17, :])], [GLR.res, WGU.res], [p.res])
            act(LA.ap[:, tt, :], p.ap, AF.Exp, [p.res], [LA.res], scale=-1.0)
            act(LA.ap[:, tt, :], LA.ap[:, tt, :], AF.Ln, [LA.res], [LA.res], bias=1.0)
        S.phase('gla_v')
        for cb2 in range(2):
            tm_proj(C_V + cb2 * 512,
                    lambda tt, p: act(VTM.ap[:, tt, cb2 * 512:(cb2 + 1) * 512], p.ap, AF.Copy, [p.res], [VTM.res]))
        S.phase('gla_qk')
        wq_res0, (wq0,) = wload([wsrc(win_d, 0, 8, C_Q, 512)])
        wq_res1, (wq1,) = wload([wsrc(win_d, 1024, 8, C_Q, 512)])

        def cumT(h):
            pb = psum()
            for c in range(4):
                S.mm(pb.ap[:, c * 128:(c + 1) * 128], [(LA.ap[:, c, h * 128:(h + 1) * 128], MASKU.ap)],
                     [LA.res, MASKU.res], [pb.res], signal=(c == 3))
            return pb

        for h in range(4):
            pb = cumT(h)
            eb = EBT[h % 2]
            act(eb.ap, pb.ap, AF.Exp, [pb.res], [eb.res], scale=-1.0 / 16)
            S.op("dve", [eb.res], [EBL.res], lambda e: e.tensor_copy(EBL.ap[:, h:16:4], eb.ap[:, 127:512:128]))
            pq = psum()
            hs = slice(h * 128, (h + 1) * 128)
            S.mm(pq.ap, [(wq0[:, k, hs], HBS.ap[:, k, :]) for k in range(8)] + [(wq1[:, k, hs], HBS.ap[:, 8 + k, :]) for k in range(8)],
                 [wq_res0, wq_res1, HBS.res], [pq.res])
            S.op("dve", [pq.res, eb.res], [QT.res],
                 lambda e: e.scalar_tensor_tensor(QT.ap[:, h, :], pq.ap, 128 ** -0.5, eb.ap, ALU.mult, ALU.mult))
        wk_res0, (wk0,) = wload([wsrc(win_d, 0, 8, C_K, 512)])
        wk_res1, (wk1,) = wload([wsrc(win_d, 1024, 8, C_K, 512)])
        for h in range(4):
            pb = cumT(h)
            enb = ENBT[h % 2]
            act(enb.ap, pb.ap, AF.Exp, [pb.res], [enb.res], scale=1.0 / 16)
            pk = psum()
            hs = slice(h * 128, (h + 1) * 128)
            S.mm(pk.ap, [(wk0[:, k, hs], HBS.ap[:, k, :]) for k in range(8)] + [(wk1[:, k, hs], HBS.ap[:, 8 + k, :]) for k in range(8)],
                 [wk_res0, wk_res1, HBS.res], [pk.res])
            S.op("dve", [pk.res, enb.res], [KT.res], lambda e: e.tensor_tensor(KT.ap[:, h, :], pk.ap, enb.ap, ALU.mult))
        S.phase('gla_ktm')
        for c in range(4):
            pbt = psum()
            S.mm(pbt.ap, [(MASKU.ap, LA.ap[:, c, :])], [LA.res, MASKU.res], [pbt.res])
            act(ENBTM.ap, pbt.ap, AF.Exp, [pbt.res], [ENBTM.res], scale=1.0 / 16)
            pk = psum()
            csl_ = slice(c * 128, (c + 1) * 128)
            S.mm(pk.ap, [(HBS.ap[:, k, csl_], wk0[:, k, :]) for k in range(8)] + [(HBS.ap[:, 8 + k, csl_], wk1[:, k, :]) for k in range(8)],
                 [wk_res0, wk_res1, HBS.res], [pk.res])
            S.op("dve", [pk.res, ENBTM.res], [KTM.res], lambda e: e.tensor_tensor(KTM.ap[:, c, :], pk.ap, ENBTM.ap, ALU.mult))
        S.phase('gla_r')

        def r_evac(ch, p):
            rt = RTMP[ch % 2]
            act(rt.ap, p.ap, AF.Silu, [p.res], [rt.res])
            S.op("dve", [rt.res, GON.res], [YGLA.res],
                 lambda e: e.tensor_scalar(YGLA.ap[:, ch, :], rt.ap, GON.ap[:, ch:ch + 1], None, ALU.mult))

        fm_proj(C_R, 8, r_evac)
        S.phase('gla_loop')
        su_gen = fm_proj_gen(C_SU, 8, lambda ch, p: act(YSG.ap[:, ch, :], p.ap, AF.Gelu, [p.res], [YSG.res]))

        def scores(c):
            csl = slice(c * 128, (c + 1) * 128)
            pa = psum()
            for h in range(4):
                S.mm(pa.ap[:, h * 128:(h + 1) * 128], [(KT.ap[:, h, csl], QT.ap[:, h, csl])], [KT.res, QT.res], [pa.res],
                     signal=(h == 3))
            pt = PT4[c % 2]
            S.op("dve", [pa.res, MASK4.res], [pt.res],
                 lambda e: e.tensor_tensor(pt.ap, pa.ap.rearrange("p (h t) -> p h t", t=128), MASK4.ap, ALU.mult))

        scores(0)
        for c in range(4):
            csl = slice(c * 128, (c + 1) * 128)
            pt = PT4[c % 2]
            pB = [psum(), psum()]
            for j in range(2):
                for h in range(4):
                    S.mm(pB[j].ap[:, h * 128:(h + 1) * 128],
                         [(VTM.ap[:, c, h * 256 + j * 128:h * 256 + (j + 1) * 128], pt.ap[:, h, :]),
                          (STB4.ap[:, h, j * 128:(j + 1) * 128], QT.ap[:, h, csl])],
                         [VTM.res, pt.res, STB4.res, QT.res], [pB[j].res], signal=(h == 3))
            pKV = [psum(), psum()]
            for h in range(4):
                S.mm(pKV[h // 2].ap[:, (h % 2) * 256:(h % 2 + 1) * 256],
                     [(KTM.ap[:, c, h * 128:(h + 1) * 128], VTM.ap[:, c, h * 256:(h + 1) * 256])],
                     [KTM.res, VTM.res], [pKV[h // 2].res], signal=(h % 2 == 1))
            if c < 3:
                scores(c + 1)
            for _ in range(2):
                next(su_gen, None)
            for b in range(2):
                sv_ = ST4.ap[:, 2 * b:2 * b + 2, :]
                S.op("dve", [pKV[b].res, ST4.res], [ST4.res],
                     lambda e: e.tensor_tensor(sv_, pKV[b].ap.rearrange("p (h e) -> p h e", e=256), sv_, ALU.add))
            eblb = EBL.ap[:, c * 4:(c + 1) * 4].unsqueeze(2).to_broadcast([128, 4, 256])
            S.op("dve", [ST4.res, EBL.res], [ST4.res], lambda e: e.tensor_tensor(ST4.ap, ST4.ap, eblb, ALU.mult))
            act(STB4.ap, ST4.ap, AF.Copy, [ST4.res], [STB4.res])
            for j in range(2):
                act(OSQ8.ap[:, j * 4:(j + 1) * 4, :], pB[j].ap.rearrange("p (h t) -> p h t", t=128), AF.Square,
                    [pB[j].res], [OSQ8.res])
            pss = psum()
            for h in range(4):
                S.mm(pss.ap[:, h * 128:(h + 1) * 128], [(ONESB.ap, OSQ8.ap[:, h, :]), (ONESB.ap, OSQ8.ap[:, 4 + h, :])],
                     [ONESB.res, OSQ8.res], [pss.res], signal=(h == 3))
            rsqrt_from_psum(RS4.ap, pss.ap.rearrange("p (h t) -> p h t", t=128), 1.0 / 256, pss.res, RS4.res)
            for j in range(2):
                S.op("dve", [pB[j].res, RS4.res], [OTT.res],
                     lambda e: e.tensor_tensor(OTT.ap[:, j * 4:(j + 1) * 4, :], pB[j].ap.rearrange("p (h t) -> p h t", t=128),
                                               RS4.ap, ALU.mult))
                yv = YGLA.ap[:, j:8:2, csl]
                S.op("dve", [OTT.res, YGLA.res], [YGLA.res],
                     lambda e: e.tensor_tensor(yv, OTT.ap[:, j * 4:(j + 1) * 4, :], yv, ALU.mult))
        S.phase('sg_u')
        for _ in su_gen:
            pass
        S.phase('sg_v')
        xq_gen = fm_proj_gen(C_XQ, 8, lambda ch, p: act(XQT.ap[:, ch, :], p.ap, AF.Copy, [p.res], [XQT.res], scale=1.0 / 16))
        for cb2 in range(2):
            lsl = slice(cb2 * 512, (cb2 + 1) * 512)
            tm_proj(C_SV + cb2 * 512, lambda tt, p: act(GV4.ap[:, tt, :], p.ap, AF.Gelu, [p.res], [GV4.res]))
            for _ in range(4):
                next(xq_gen, None)
            for tt in range(4):
                for gi in range(2):
                    S.op("dve", [GV4.res], [BNS8.res],
                         lambda e: e.bn_stats(BNS8.ap[:, tt * 2 + gi, :], GV4.ap[:, tt, gi * 256:(gi + 1) * 256]))
            for q8 in range(8):
                S.op("dve", [BNS8.res], [BNA8.res], lambda e: e.bn_aggr(BNA8.ap[:, q8, :], BNS8.ap[:, q8, :]))
            act(LNR8.ap, BNA8.ap[:, :, 1], AF.Ln, [BNA8.res], [LNR8.res], bias=EPS)
            act(LNR8.ap, LNR8.ap, AF.Exp, [LNR8.res], [LNR8.res], scale=-0.5)
            for tt in range(4):
                for gi in range(2):
                    gsl = slice(gi * 256, (gi + 1) * 256)
                    q8 = tt * 2 + gi
                    S.op("dve", [GV4.res, BNA8.res, LNR8.res], [GV4.res],
                         lambda e: e.tensor_scalar(GV4.ap[:, tt, gsl], GV4.ap[:, tt, gsl], BNA8.ap[:, q8, 0:1], LNR8.ap[:, q8:q8 + 1],
                                                   ALU.subtract, ALU.mult))
            lg = LNBC.ap[:, lsl].unsqueeze(1).to_broadcast([128, 4, 512])
            lb = LNBC.ap[:, 1024 + cb2 * 512:1024 + (cb2 + 1) * 512].unsqueeze(1).to_broadcast([128, 4, 512])
            S.op("dve", [GV4.res, LNBC.res], [GV4.res], lambda e: e.tensor_tensor(GV4.ap, GV4.ap, lg, ALU.mult))
            S.op("dve", [GV4.res, LNBC.res], [SVTM.res], lambda e: e.tensor_tensor(SVTM.ap[:, :, lsl], GV4.ap, lb, ALU.add))
        S.phase('sg_mix')
        for g in range(4):
            for cj in range(2):
                p = psum()
                for tt in range(4):
                    S.mm(p.ap[:, tt * 128:(tt + 1) * 128],
                         [(SVTM.ap[:, tt, g * 256 + cj * 128:g * 256 + (cj + 1) * 128], WST.ap[:, g, :]),
                          (ONESF.ap[0:1, :], BSROW.ap[0:1, g * 128:(g + 1) * 128])],
                         [SVTM.res, WST.res, ONESF.res, BSROW.res], [p.res], signal=(tt == 3))
                yv = YSG.ap[:, g * 2 + cj, :]
                S.op("dve", [p.res, YSG.res], [YSG.res], lambda e: e.tensor_tensor(yv, p.ap, yv, ALU.mult))
        S.phase('xa_q')
        for _ in xq_gen:
            pass
        S.phase('xa_attn')
        def xa_scores(tt):
            tsl_ = slice(tt * 128, (tt + 1) * 128)
            psc_ = [psum(), psum()]
            for h in range(4):
                S.mm(psc_[h // 2].ap[:, (h % 2) * 256:(h % 2 + 1) * 256],
                     [(XQT.ap[:, h * 2 + j, tsl_], MKT.ap[:, h * 2 + j, :]) for j in range(2)],
                     [XQT.res, MKT.res], [psc_[h // 2].res], signal=(h % 2 == 1))
            return psc_

        psc_next = xa_scores(0)
        for tt in range(4):
            tsl = slice(tt * 128, (tt + 1) * 128)
            pf, pt2 = PF[tt % 2], PT2[tt % 2]
            psc = psc_next
            if tt < 3:
                psc_next = xa_scores(tt + 1)
            for b in range(2):
                S.op("dve", [psc[b].res], [MXB.res],
                     lambda e: e.tensor_reduce(MXB.ap[:, 2 * b:2 * b + 2], psc[b].ap.rearrange("p (h m) -> p h m", m=256), AX.X, ALU.max))
            S.op("dve", [MXB.res], [NMX.res], lambda e: e.tensor_scalar(NMX.ap, MXB.ap, -1.0, None, ALU.mult))
            for h in range(4):
                act(pf.ap[:, h, :], psc[h // 2].ap[:, (h % 2) * 256:(h % 2 + 1) * 256], AF.Exp,
                    [psc[h // 2].res, NMX.res], [pf.res, RSUM.res], bias=NMX.ap[:, h:h + 1], accum_out=RSUM.ap[:, h:h + 1])
            S.op("dve", [RSUM.res], [RINV.res], lambda e: e.reciprocal(RINV.ap, RSUM.ap))
            for h in range(4):
                S.op("dve", [pf.res, RINV.res], [pf.res],
                     lambda e: e.tensor_scalar(pf.ap[:, h, :], pf.ap[:, h, :], RINV.ap[:, h:h + 1], None, ALU.mult))
            for b in range(2):
                ptp = psum()
                for q in range(4):
                    h, mt = b * 2 + q // 2, q % 2
                    S.tr(ptp.ap[:, q * 128:(q + 1) * 128], pf.ap[:, h, mt * 128:(mt + 1) * 128], IDENT.ap,
                         [pf.res, IDENT.res], [ptp.res], signal=(q == 3))
                ov = pt2.ap[:, b * 4:(b + 1) * 4, :]
                iv = ptp.ap.rearrange("p (a b) -> p a b", b=128)
                if b == 0:
                    act(ov, iv, AF.Copy, [ptp.res], [pt2.res])
                else:
                    S.op("dve", [ptp.res], [pt2.res], lambda e: e.tensor_copy(ov, iv))
            for b in range(2):
                po = psum()
                for q in range(4):
                    hj = b * 4 + q
                    h, j = hj // 2, hj % 2
                    S.mm(po.ap[:, q * 128:(q + 1) * 128],
                         [(MV.ap[:, mt, h * 256 + j * 128:h * 256 + (j + 1) * 128], pt2.ap[:, h * 2 + mt, :]) for mt in range(2)],
                         [MV.res, pt2.res], [po.res], signal=(q == 3))
                ov = YXA.ap[:, b * 4:(b + 1) * 4, tsl]
                iv = po.ap.rearrange("p (a b) -> p a b", b=128)
                if b == 0:
                    act(ov, iv, AF.Copy, [po.res], [YXA.res])
                else:
                    S.op("dve", [po.res], [YXA.res], lambda e: e.tensor_copy(ov, iv))
        ys = [YGLA, YSG, YXA]
        S.phase('merge')
        for jg in range(4):
            mrg = MRG[jg % 2]
            for i in range(3):
                gt = GT[i % 2]
                gbanks = [psum() for _ in range(4)]
                for kh in range(2):
                    wres, (wv,) = wload([wsrc(win_d, kh * 1024, 8, C_GATE + i * 2048 + jg * 512, 512)])
                    for ji in range(4):
                        p = gbanks[ji]
                        S.mm(p.ap, [(wv[:, k, ji * 128:(ji + 1) * 128], HBS.ap[:, kh * 8 + k, :]) for k in range(8)],
                             [wres, HBS.res], [p.res], start=(kh == 0), stop=(kh == 1))
                        if kh == 1:
                            act(gt.ap[:, ji, :], p.ap, AF.Sigmoid, [p.res], [gt.res])
                wres, (wv,) = wload([wsrc(wbr_d, i * 1024, 8, jg * 512, 512)])
                for ji in range(4):
                    p = psum()
                    S.mm(p.ap, [(wv[:, k, ji * 128:(ji + 1) * 128], ys[i].ap[:, k, :]) for k in range(8)], [wres, ys[i].res], [p.res])
                    if i == 0:
                        S.op("dve", [p.res, gt.res], [ACC.res], lambda e: e.tensor_tensor(ACC.ap[:, ji, :], gt.ap[:, ji, :], p.ap, ALU.mult))
                    else:
                        tt_ = TT_[ji % 2]
                        S.op("dve", [p.res, gt.res], [tt_.res], lambda e: e.tensor_tensor(tt_.ap, gt.ap[:, ji, :], p.ap, ALU.mult))
                        if i == 1:
                            S.op("dve", [tt_.res, ACC.res], [ACC.res], lambda e: e.tensor_tensor(ACC.ap[:, ji, :], ACC.ap[:, ji, :], tt_.ap, ALU.add))
                        else:
                            S.op("dve", [tt_.res, ACC.res], [mrg.res], lambda e: e.tensor_tensor(mrg.ap[:, ji, :], ACC.ap[:, ji, :], tt_.ap, ALU.add))
            for oh in range(2):
                wres, (wv,) = wload([wsrc(wout_d, jg * 512, 4, oh * 1024, 1024)])
                for d8 in range(8):
                    dc = oh * 8 + d8
                    p = psum()
                    S.mm(p.ap, [(wv[:, k, d8 * 128:(d8 + 1) * 128], mrg.ap[:, k, :]) for k in range(4)], [wres, mrg.res], [p.res])
                    xv = X.ap[:, dc, t0:t0 + 512]
                    S.op("dve", [p.res, X.res], [X.res], lambda e: e.tensor_tensor(xv, p.ap, xv, ALU.add))

    for tile in range(2):
        row0 = tile * 1024
        S.phase('load_x')
        load_fm(lambda tt: x_d[row0 + tt * 128: row0 + (tt + 1) * 128, :], X, 8, STG)
        ffn(*ffw[0], 0)
        for sub in range(2):
            mixer(sub * 512)
        ffn(*ffw[1], 32)
        S.phase('final_norm')
        rmsnorm(X, 0, X, 0, 48, 1024, E_OFF)
        S.phase('store')
        store_fm(X, row0, 8, STG)
    for k in ("out0", "out1"):
        nc.sync.wait_ge(S.sem[k], S.cnt[k])
    S.phase('end')
    nc._phases = S.phases
    return nc


_NC_CACHE = {}


def _fm(v):
    return np.ascontiguousarray(np.asarray(v, np.float32).reshape(16, 128).T)


def kernel(x, mem, ffn1_norm, ffn1_w_gate, ffn1_w_up, ffn1_w_down, mix_norm, mem_norm,
           w_in, gla_w_gate_up, gla_gate_bias, gla_out_norm, sg_ln_g, sg_ln_b, sg_w_s, sg_b_s,
           w_kv_mem, w_branch, w_out, ffn2_norm, ffn2_w_gate, ffn2_w_up, ffn2_w_down, final_norm):
    f = lambda a: np.ascontiguousarray(np.asarray(a, np.float32))
    x = f(x)
    mem = f(mem)
    B = x.shape[0]
    gains = np.concatenate([_fm(f(ffn1_norm)[0]), _fm(f(mix_norm)[0]), _fm(f(ffn2_norm)[0]),
                            _fm(f(final_norm)), _fm(f(mem_norm)[0])], axis=1)
    gon = np.ascontiguousarray(f(gla_out_norm)[0].reshape(8, 128).T)
    lnbc = np.ascontiguousarray(np.broadcast_to(
        np.concatenate([f(sg_ln_g)[0].reshape(-1), f(sg_ln_b)[0].reshape(-1)])[None, :], (128, 2048)))
    wst = np.ascontiguousarray(f(sg_w_s)[0].transpose(2, 0, 1).reshape(128, 512))
    wgu = np.ascontiguousarray(np.concatenate([f(gla_w_gate_up)[0], f(gla_gate_bias)[0][None, :]], axis=0))
    bs = np.ascontiguousarray(f(sg_b_s)[0].reshape(1, 512))
    cst = np.ascontiguousarray(np.concatenate([np.eye(128, dtype=np.float32),
                                               np.triu(np.ones((128, 128), np.float32))], axis=1))
    shared = {
        "w_in": f(w_in)[0], "f1g": f(ffn1_w_gate)[0], "f1u": f(ffn1_w_up)[0], "f1d": f(ffn1_w_down)[0],
        "f2g": f(ffn2_w_gate)[0], "f2u": f(ffn2_w_up)[0], "f2d": f(ffn2_w_down)[0],
        "w_kv": f(w_kv_mem)[0], "w_br": f(w_branch)[0].reshape(3072, 2048), "w_out": f(w_out)[0],
        "gains": gains, "gon": gon, "lnbc": lnbc, "wst": wst, "wgu": wgu, "bs": bs, "cst": cst,
    }
    if "nc" not in _NC_CACHE:
        _NC_CACHE["nc"] = build_nc()
    nc = _NC_CACHE["nc"]
    in_maps = []
    for b in range(B):
        m = dict(shared)
        m["x"] = x[b]
        m["mem"] = mem[b]
        in_maps.append(m)
    res = run_bass_kernel_spmd(nc, in_maps, core_ids=list(range(B)))
    return np.stack([np.asarray(r["y"], np.float32) for r in res.results], axis=0)
```

```python
import numpy as np
import concourse.bass as bass
import concourse.mybir as mybir
from concourse.bass_utils import run_bass_kernel_spmd

F32 = mybir.dt.float32
BF16 = mybir.dt.bfloat16
AF = mybir.ActivationFunctionType
ALU = mybir.AluOpType
AX = mybir.AxisListType

D = 2048
SEQ = 2048
DFF = 5632
EPS = 1e-6
C_Q, C_K, C_V, C_R, C_GLR, C_SU, C_SV, C_XQ, C_GATE = 0, 512, 1024, 2048, 3072, 3088, 4112, 5136, 6160


class Res:
    __slots__ = ("name", "lo", "hi", "w", "r", "al")

    def __init__(self, name, lo=None, hi=None):
        self.name, self.lo, self.hi = name, lo, hi
        self.w = None
        self.r = {}
        self.al = []


class Buf:
    __slots__ = ("ap", "res")

    def __init__(self, ap, res):
        self.ap, self.res = ap, res


class Sched:
    def __init__(self, nc):
        self.nc = nc
        self.eng = {"pe": nc.tensor, "act": nc.scalar, "dve": nc.vector, "pool": nc.gpsimd, "sp": nc.sync}
        self.sem = {}
        self.cnt = {}
        self.seen = {e: {} for e in self.eng}
        for e in self.eng:
            self.sem[e] = nc.alloc_semaphore("s_" + e)
            self.cnt[e] = 0
        self.all_res = []
        self.npe = 0
        self.phases = []

    def phase(self, name):
        self.phases.append((name, self.npe))

    def _sync(self, e, reads, writes):
        need = {}

        def add(ev):
            if ev is None:
                return
            k, v = ev
            if need.get(k, 0) < v:
                need[k] = v

        for r in reads:
            add(r.w)
            for t in r.al:
                add(t.w)
        for w in writes:
            for t in [w] + w.al:
                for k, v in t.r.items():
                    add((k, v))
                add(t.w)
        seen = self.seen[e]
        eng = self.eng[e]
        for k, v in need.items():
            if e == "pe" and k == "pe":
                continue
            if seen.get(k, 0) >= v:
                continue
            eng.wait_ge(self.sem[k], v)
            seen[k] = v

    def _post(self, key, val, reads, writes):
        for r in reads:
            r.r[key] = val
        for w in writes:
            w.w = (key, val)
            w.r = {}

    def op(self, e, reads, writes, fn):
        self._sync(e, reads, writes)
        ins = fn(self.eng[e])
        self.cnt[e] += 1
        ins.then_inc(self.sem[e], 1)
        self._post(e, self.cnt[e], reads, writes)

    def mm(self, out_ap, pairs, reads, writes, start=True, stop=True, signal=True):
        self._sync("pe", reads, writes)
        n = len(pairs)
        ins = None
        self.npe += n
        for i, (l, r) in enumerate(pairs):
            ins = self.nc.tensor.matmul(out_ap, lhsT=l, rhs=r, start=(start and i == 0), stop=(stop and i == n - 1))
        if signal:
            self.cnt["pe"] += 1
            ins.then_inc(self.sem["pe"], 1)
            self._post("pe", self.cnt["pe"], reads, writes)
        else:
            self._post("pe", self.cnt["pe"] + 1, reads, writes)

    def tr(self, out_ap, in_ap, ident_ap, reads, writes, signal=True):
        self._sync("pe", reads, writes)
        self.npe += 1
        ins = self.nc.tensor.transpose(out_ap, in_ap, ident_ap)
        if signal:
            self.cnt["pe"] += 1
            ins.then_inc(self.sem["pe"], 1)
            self._post("pe", self.cnt["pe"], reads, writes)
        else:
            self._post("pe", self.cnt["pe"] + 1, reads, writes)

    def dma(self, q, key, parts, reads, writes):
        if key not in self.sem:
            self.sem[key] = self.nc.alloc_semaphore("d_" + key)
            self.cnt[key] = 0
        self._sync(q, reads, writes)
        for (o, i) in parts:
            ins = self.eng[q].dma_start(out=o, in_=i)
            self.cnt[key] += 16
            ins.then_inc(self.sem[key], 16)
        self._post(key, self.cnt[key], reads, writes)


def build_nc():
    nc = bass.Bass("TRN2", target_bir_lowering=False)
    S = Sched(nc)

    def din(name, shape):
        return nc.dram_tensor(name, list(shape), F32, kind="ExternalInput").ap()

    x_d = din("x", [SEQ, D])
    mem_d = din("mem", [256, D])
    win_d = din("w_in", [D, 12304])
    ffw = []
    for i in (1, 2):
        ffw.append((din(f"f{i}g", [D, DFF]), din(f"f{i}u", [D, DFF]), din(f"f{i}d", [DFF, D])))
    wkv_d = din("w_kv", [D, D])
    wbr_d = din("w_br", [3072, D])
    wout_d = din("w_out", [D, D])
    gains_d = din("gains", [128, 80])
    gon_d = din("gon", [128, 8])
    lnbc_d = din("lnbc", [128, 2048])
    wst_d = din("wst", [128, 512])
    wgu_d = din("wgu", [17, 512])
    bs_d = din("bs", [1, 512])
    cst_d = din("cst", [128, 256])
    y_d = nc.dram_tensor("y", [SEQ, D], F32, kind="ExternalOutput").ap()

    TOTAL = 212480
    BIG = nc.alloc_sbuf_tensor("BIG", [128, TOTAL // 2], BF16)

    def carve(name, off, shape, dtype):
        esz = 2 if dtype == BF16 else 4
        n = 1
        for s_ in shape:
            n *= s_
        assert off % 4 == 0 and off + n * esz <= TOTAL, (name, off, n * esz)
        ap = BIG[:, off // 2: off // 2 + n * esz // 2]
        if dtype == F32:
            ap = ap.bitcast(F32)
        if len(shape) == 2:
            ap = ap.rearrange("p (a b) -> p a b", b=shape[1])
        res = Res(name, off, off + n * esz)
        for o in S.all_res:
            if o.lo < res.hi and res.lo < o.hi:
                o.al.append(res)
                res.al.append(o)
        S.all_res.append(res)
        return Buf(ap, res)

    X_OFF = 0
    HB_OFF = 65536
    W_OFF = 98304
    R_OFF = 131072
    E_OFF = 153600
    BS_OFF = 180224
    C_OFF = 182272
    MKT_OFF = 196608
    MV_OFF = 200704
    ST_OFF = 204800
    STB_OFF = 208896
    SM_OFF = 210944
    S1 = R_OFF
    S2 = HB_OFF + 16384

    X = carve("X", X_OFF, [16, 1024], F32)
    HB = carve("HB", HB_OFF, [16, 1024], BF16)
    HBS = carve("HBS", HB_OFF, [16, 512], BF16)
    WS = [carve(f"W{i}", W_OFF + i * 8192, [4096], BF16) for i in range(4)]
    H1 = carve("H1", R_OFF, [11, 1024], BF16)
    STG = [carve(f"STG{i}", R_OFF + i * 8192, [2048], F32) for i in range(2)]
    SGT = [carve(f"SGT{i}", E_OFF + i * 1024, [512], BF16) for i in range(4)]
    o = C_OFF
    GAINS = carve("GAINS", o, [80], F32); o += 320
    GON = carve("GON", o, [8], F32); o += 32
    IDENT = carve("IDENT", o, [128], F32); o += 512
    MASKU = carve("MASKU", o, [128], F32); o += 512
    ONESB = carve("ONESB", o, [128], BF16); o += 256
    ONESF = carve("ONESF", o, [128], F32); o += 512
    WGU = carve("WGU", o, [512], F32); o += 2048
    WST = carve("WST", o, [4, 128], BF16); o += 1024
    WGLR = carve("WGLR", o, [16, 16], BF16); o += 512
    LNBC = carve("LNBC", o, [2048], F32); o += 8192
    assert o <= C_OFF + 14336
    BSROW = carve("BSROW", BS_OFF, [512], F32)
    MKT = carve("MKT", MKT_OFF, [8, 256], BF16)
    MV = carve("MV", MV_OFF, [2, 1024], BF16)
    ST = [carve(f"ST{h}", ST_OFF + h * 1024, [256], F32) for h in range(4)]
    STB = [carve(f"STB{h}", STB_OFF + h * 512, [256], BF16) for h in range(4)]
    o = SM_OFF
    EBL = carve("EBL", o, [16], F32); o += 64
    BNS = carve("BNS", o, [2, 6], F32); o += 48
    BNA = carve("BNA", o, [2, 2], F32); o += 16
    LNR = carve("LNR", o, [2], F32); o += 8
    o += 8
    MXB = carve("MXB", o, [4], F32); o += 16
    NMX = carve("NMX", o, [4], F32); o += 16
    RSUM = carve("RSUM", o, [4], F32); o += 16
    RINV = carve("RINV", o, [4], F32); o += 16
    assert o <= TOTAL
    QT = carve("QT", S1 + 0, [4, 512], BF16)
    KT = carve("KT", S1 + 4096, [4, 512], BF16)
    KTM = carve("KTM", S1 + 8192, [4, 512], BF16)
    VTM = carve("VTM", S1 + 12288, [4, 1024], BF16)
    GLR = carve("GLR", S1 + 20480, [512], F32)
    LA = carve("LA", S2 + 0, [4, 512], F32)
    EBT = [carve(f"EBT{i}", S2 + 8192 + i * 2048, [512], F32) for i in range(2)]
    ENBT = [carve(f"ENBT{i}", S2 + 12288 + i * 2048, [512], F32) for i in range(2)]
    YGLA = carve("YGLA", E_OFF, [8, 512], BF16)
    YSG = carve("YSG", E_OFF + 8192, [8, 512], BF16)
    YXA = carve("YXA", E_OFF + 16384, [8, 512], BF16)
    ENBTM = carve("ENBTM", E_OFF + 24576, [512], F32)
    PT4 = [carve(f"PT4_{i}", S2 + i * 1024, [4, 128], BF16) for i in range(2)]
    OSQ8 = carve("OSQ8", S2 + 2048, [8, 128], BF16)
    RS4 = carve("RS4", S2 + 4096, [4, 128], F32)
    OTT = carve("OTT", S2 + 6144, [8, 128], F32)
    RTMP = [carve(f"RTMP{i}", S2 + 10240 + i * 2048, [512], F32) for i in range(2)]
    MASK4 = carve("MASK4", SM_OFF + 256, [4, 128], BF16)
    ST4 = carve("ST4", ST_OFF, [4, 256], F32)
    STB4 = carve("STB4", STB_OFF, [4, 256], BF16)
    GV4 = carve("GV4", S2, [4, 512], F32)
    BNS8 = carve("BNS8", SM_OFF + 1280, [8, 6], F32)
    BNA8 = carve("BNA8", SM_OFF + 1280 + 192, [8, 2], F32)
    LNR8 = carve("LNR8", SM_OFF + 208, [8], F32)
    SVTM = carve("SVTM", S1 + 0, [4, 1024], BF16)
    XQT = carve("XQT", S1 + 8192, [8, 512], BF16)
    PF = [carve(f"PF{i}", S2 + 8192 + i * 4096, [4, 256], F32) for i in range(2)]
    PT2 = [carve(f"PT2{i}", S1 + 16384 + i * 2048, [8, 128], BF16) for i in range(2)]
    ACC = carve("ACC", S2 + 0, [4, 512], F32)
    GT = [carve(f"GT{i}", S2 + 8192 + i * 4096, [4, 512], BF16) for i in range(2)]
    MRG = [carve(f"MRG{i}", S1 + i * 4096, [4, 512], BF16) for i in range(2)]
    TT_ = [carve(f"TT{i}", S1 + 8192 + i * 2048, [512], F32) for i in range(2)]

    PS = []
    for i in range(8):
        t = nc.alloc_psum_tensor(f"ps{i}", [128, 512], F32)
        PS.append(Buf(t[:, :], Res(f"ps{i}")))
    state = {"pi": 0, "wi": 0}

    def psum():
        b = PS[state["pi"] % 8]
        state["pi"] += 1
        return b

    def wsrc(d2, r0, nk, c0, ncol):
        return d2[r0:r0 + nk * 128, c0:c0 + ncol].rearrange("(k p) c -> p k c", p=128)

    def wload(srcs):
        idx = state["wi"] % 4
        state["wi"] += 1
        slot = WS[idx]
        off = 0
        views, parts = [], []
        for s_ in srcs:
            nk, ncol = s_.shape[1], s_.shape[2]
            v = slot.ap[:, off:off + nk * ncol].rearrange("p (k c) -> p k c", c=ncol)
            parts.append((v, s_))
            views.append(v)
            off += nk * ncol
        assert off <= 4096
        S.dma("pool", f"w{idx}", parts, [], [slot.res])
        return slot.res, views

    def act(out, in_, func, reads, writes, **kw):
        S.op("act", reads, writes, lambda e: e.activation(out, in_, func, **kw))

    def rsqrt_from_psum(dst, ps_ap, scale, reads_ps, dres):
        act(dst, ps_ap, AF.Ln, [reads_ps], [dres], bias=EPS, scale=scale)
        act(dst, dst, AF.Exp, [dres], [dres], scale=-0.5)

    def rmsnorm(src, s_t0, dst, d_t0, gcol, ntok, tmp_off, blk=512):
        SQ = [carve(f"sq{tmp_off}_{i}", tmp_off + i * 1024, [512], BF16) for i in range(4)]
        RS = carve(f"rs{tmp_off}", tmp_off + 4096, [512], F32)
        for tb in range(ntok // blk):
            ssl = slice(s_t0 + tb * blk, s_t0 + (tb + 1) * blk)
            dsl = slice(d_t0 + tb * blk, d_t0 + (tb + 1) * blk)
            p = psum()
            for c in range(16):
                sq = SQ[c % 4]
                if c % 2 == 0:
                    act(sq.ap[:, 0:blk], src.ap[:, c, ssl], AF.Square, [src.res], [sq.res])
                else:
                    S.op("dve", [src.res], [sq.res],
                         lambda e: e.tensor_tensor(sq.ap[:, 0:blk], src.ap[:, c, ssl], src.ap[:, c, ssl], ALU.mult))
                S.mm(p.ap[:, 0:blk], [(ONESB.ap, sq.ap[:, 0:blk])], [sq.res, ONESB.res], [p.res],
                     start=(c == 0), stop=(c == 15))
            rsqrt_from_psum(RS.ap[:, 0:blk], p.ap[:, 0:blk], 1.0 / D, p.res, RS.res)
            for c in range(16):
                S.op("dve", [src.res, RS.res, GAINS.res], [dst.res],
                     lambda e: e.scalar_tensor_tensor(dst.ap[:, c, dsl], src.ap[:, c, ssl],
                                                      GAINS.ap[:, gcol + c:gcol + c + 1], RS.ap[:, 0:blk],
                                                      ALU.mult, ALU.mult))

    def load_fm(dram_rows, dst, ntt, stg):
        for tt in range(ntt):
            st = stg[tt % 2]
            S.dma("sp", f"stg{tt % 2}", [(st.ap, dram_rows(tt))], [], [st.res])
            for cb in range(4):
                p = psum()
                for ci in range(4):
                    c = cb * 4 + ci
                    S.tr(p.ap[:, ci * 128:(ci + 1) * 128], st.ap[:, c * 128:(c + 1) * 128], IDENT.ap,
                         [st.res, IDENT.res], [p.res], signal=(ci == 3))
                ov = dst.ap[:, cb * 4:cb * 4 + 4, tt * 128:(tt + 1) * 128]
                iv = p.ap.rearrange("p (a b) -> p a b", b=128)
                if cb % 2 == 0:
                    act(ov, iv, AF.Copy, [p.res], [dst.res])
                else:
                    S.op("dve", [p.res], [dst.res], lambda e: e.tensor_copy(ov, iv))

    def store_fm(src, row0, ntt, stg):
        for tt in range(ntt):
            st = stg[tt % 2]
            for cb in range(4):
                p = psum()
                for ci in range(4):
                    c = cb * 4 + ci
                    S.tr(p.ap[:, ci * 128:(ci + 1) * 128], src.ap[:, c, tt * 128:(tt + 1) * 128], IDENT.ap,
                         [src.res, IDENT.res], [p.res], signal=(ci == 3))
                ov = st.ap[:, cb * 512:(cb + 1) * 512]
                if cb % 2 == 0:
                    act(ov, p.ap, AF.Copy, [p.res], [st.res])
                else:
                    S.op("dve", [p.res], [st.res], lambda e: e.tensor_copy(ov, p.ap))
            S.dma("sp", f"out{tt % 2}", [(y_d[row0 + tt * 128: row0 + (tt + 1) * 128, :], st.ap)], [st.res], [])

    cparts = [
        (GAINS.ap, gains_d), (GON.ap, gon_d), (IDENT.ap, cst_d[:, 0:128]), (MASKU.ap, cst_d[:, 128:256]),
        (WGU.ap[0:17, :], wgu_d), (BSROW.ap[0:1, :], bs_d), (LNBC.ap, lnbc_d),
    ]
    S.dma("sp", "cst", cparts, [], [GAINS.res, GON.res, IDENT.res, MASKU.res, WGU.res, BSROW.res, LNBC.res])
    S.dma("pool", "wglr", [(WGLR.ap, wsrc(win_d, 0, 16, C_GLR, 16))], [], [WGLR.res])
    S.op("dve", [], [ONESB.res], lambda e: e.memset(ONESB.ap, 1.0))
    S.op("dve", [], [ONESF.res], lambda e: e.memset(ONESF.ap, 1.0))
    for h in range(4):
        S.op("dve", [], [ST[h].res], lambda e: e.memset(ST[h].ap, 0.0))
        S.op("dve", [], [STB[h].res], lambda e: e.memset(STB[h].ap, 0.0))
    S.op("dve", [], [GLR.res], lambda e: e.memset(GLR.ap[0:32, :], 1.0))
    WTMP = carve("WTMP", E_OFF, [512], F32)
    S.dma("sp", "cst2", [(WTMP.ap, wst_d)], [], [WTMP.res])
    for g in range(4):
        S.op("dve", [WTMP.res, MASKU.res], [WST.res],
             lambda e: e.tensor_tensor(WST.ap[:, g, :], WTMP.ap[:, g * 128:(g + 1) * 128], MASKU.ap, ALU.mult))

    for h in range(4):
        S.op("dve", [MASKU.res], [MASK4.res], lambda e: e.tensor_copy(MASK4.ap[:, h, :], MASKU.ap))
    S.phase('mem_init')
    MX_ = carve("MEMX", R_OFF, [16, 256], F32)
    MN_ = carve("MEMN", E_OFF + 16384, [16, 256], BF16)
    MSTG = [carve(f"MSTG{i}", E_OFF + i * 8192, [2048], F32) for i in range(2)]
    load_fm(lambda tt: mem_d[tt * 128:(tt + 1) * 128, :], MX_, 2, MSTG)
    rmsnorm(MX_, 0, MN_, 0, 64, 256, HB_OFF, blk=256)
    for cb in range(0, 8, 2):
        wres, (wv,) = wload([wsrc(wkv_d, 0, 16, cb * 128, 256)])
        for ci in range(2):
            p = psum()
            S.mm(p.ap[:, 0:256], [(wv[:, k, ci * 128:(ci + 1) * 128], MN_.ap[:, k, :]) for k in range(16)],
                 [wres, MN_.res], [p.res])
            act(MKT.ap[:, cb + ci, :], p.ap[:, 0:256], AF.Copy, [p.res], [MKT.res])
    for cb2 in range(2):
        banks = [psum() for _ in range(2)]
        for kh in range(2):
            wres, (wv,) = wload([wsrc(wkv_d, kh * 1024, 8, 1024 + cb2 * 512, 512)])
            for mt in range(2):
                p = banks[mt]
                S.mm(p.ap, [(MN_.ap[:, kh * 8 + k, mt * 128:(mt + 1) * 128], wv[:, k, :]) for k in range(8)],
                     [wres, MN_.res], [p.res], start=(kh == 0), stop=(kh == 1))
                if kh == 1:
                    S.op("dve", [p.res], [MV.res], lambda e: e.tensor_copy(MV.ap[:, mt, cb2 * 512:(cb2 + 1) * 512], p.ap))

    def ffn(wg_d, wu_d, wd_d, gcol):
        S.phase('ffn_norm')
        rmsnorm(X, 0, HB, 0, gcol, 1024, R_OFF)
        for g in range(4):
            S.phase(f'ffn_gu{g}')
            for f0 in range(0, 11, 2):
                nf = min(2, 11 - f0)
                c0 = (g * 11 + f0) * 128
                wres, (wgv,) = wload([wsrc(wg_d, 0, 16, c0, nf * 128)])
                for fi in range(nf):
                    for tb in range(2):
                        p = psum()
                        S.mm(p.ap, [(wgv[:, k, fi * 128:(fi + 1) * 128], HB.ap[:, k, tb * 512:(tb + 1) * 512])
                                    for k in range(16)], [wres, HB.res], [p.res])
                        sg = SGT[fi * 2 + tb]
                        act(sg.ap, p.ap, AF.Silu, [p.res], [sg.res])
                wres, (wuv,) = wload([wsrc(wu_d, 0, 16, c0, nf * 128)])
                for fi in range(nf):
                    f = f0 + fi
                    for tb in range(2):
                        p = psum()
                        S.mm(p.ap, [(wuv[:, k, fi * 128:(fi + 1) * 128], HB.ap[:, k, tb * 512:(tb + 1) * 512])
                                    for k in range(16)], [wres, HB.res], [p.res])
                        sg = SGT[fi * 2 + tb]
                        S.op("dve", [sg.res, p.res], [H1.res],
                             lambda e: e.tensor_tensor(H1.ap[:, f, tb * 512:(tb + 1) * 512], sg.ap, p.ap, ALU.mult))
            S.phase(f'ffn_dn{g}')
            for db in range(4):
                banks = [psum() for _ in range(8)]
                for (k0, nk) in ((0, 6), (6, 5)):
                    wres, (wdv,) = wload([wsrc(wd_d, (g * 11 + k0) * 128, nk, db * 512, 512)])
                    for dc in range(4):
                        for tb in range(2):
                            p = banks[dc * 2 + tb]
                            S.mm(p.ap, [(wdv[:, k, dc * 128:(dc + 1) * 128], H1.ap[:, k0 + k, tb * 512:(tb + 1) * 512])
                                        for k in range(nk)], [wres, H1.res], [p.res], start=(k0 == 0), stop=(k0 == 6))
                            if k0 == 6:
                                xv = X.ap[:, db * 4 + dc, tb * 512:(tb + 1) * 512]
                                S.op("dve", [p.res, X.res], [X.res],
                                     lambda e: e.scalar_tensor_tensor(xv, p.ap, 0.5, xv, ALU.mult, ALU.add))

    def fm_proj(col0, nchunks, evac):
        for cb in range(0, nchunks, 2):
            n = min(2, nchunks - cb)
            wres, (wv,) = wload([wsrc(win_d, 0, 16, col0 + cb * 128, n * 128)])
            for ci in range(n):
                p = psum()
                S.mm(p.ap, [(wv[:, k, ci * 128:(ci + 1) * 128], HBS.ap[:, k, :]) for k in range(16)],
                     [wres, HBS.res], [p.res])
                evac(cb + ci, p)

    def fm_proj_gen(col0, nchunks, evac):
        for cb in range(0, nchunks, 2):
            n = min(2, nchunks - cb)
            wres, (wv,) = wload([wsrc(win_d, 0, 16, col0 + cb * 128, n * 128)])
            for ci in range(n):
                p = psum()
                S.mm(p.ap, [(wv[:, k, ci * 128:(ci + 1) * 128], HBS.ap[:, k, :]) for k in range(16)],
                     [wres, HBS.res], [p.res])
                evac(cb + ci, p)
                yield

    def tm_proj(col0, evac):
        banks = [psum() for _ in range(4)]
        for kh in range(2):
            wres, (wv,) = wload([wsrc(win_d, kh * 1024, 8, col0, 512)])
            for tt in range(4):
                p = banks[tt]
                S.mm(p.ap, [(HBS.ap[:, kh * 8 + k, tt * 128:(tt + 1) * 128], wv[:, k, :]) for k in range(8)],
                     [wres, HBS.res], [p.res], start=(kh == 0), stop=(kh == 1))
                if kh == 1:
                    evac(tt, p)

    def mixer(t0):
        S.phase('mix_norm')
        rmsnorm(X, t0, HBS, 0, 16, 512, S2)
        S.phase('gla_glr_z')
        p = psum()
        S.mm(p.ap[0:16, :], [(WGLR.ap[:, k, :], HBS.ap[:, k, :]) for k in range(16)], [WGLR.res, HBS.res], [p.res])
        S.op("dve", [], [GLR.res], lambda e: e.memset(GLR.ap[0:32, :], 1.0))
        act(GLR.ap[0:16, :], p.ap[0:16, :], AF.Copy, [p.res], [GLR.res])
        for tt in range(4):
            p = psum()
            S.mm(p.ap, [(GLR.ap[0:17, tt * 128:(tt + 1) * 128], WGU.ap[0:17, :])], [GLR.res, WGU.res], [p.res])
            act(LA.ap[:, tt, :], p.ap, AF.Exp, [p.res], [LA.res], scale=-1.0)
            act(LA.ap[:, tt, :], LA.ap[:, tt, :], AF.Ln, [LA.res], [LA.res], bias=1.0)
        S.phase('gla_v')
        for cb2 in range(2):
            tm_proj(C_V + cb2 * 512,
                    lambda tt, p: act(VTM.ap[:, tt, cb2 * 512:(cb2 + 1) * 512], p.ap, AF.Copy, [p.res], [VTM.res]))
        S.phase('gla_qk')
        wq_res0, (wq0,) = wload([wsrc(win_d, 0, 8, C_Q, 512)])
        wq_res1, (wq1,) = wload([wsrc(win_d, 1024, 8, C_Q, 512)])

        def cumT(h):
            pb = psum()
            for c in range(4):
                S.mm(pb.ap[:, c * 128:(c + 1) * 128], [(LA.ap[:, c, h * 128:(h + 1) * 128], MASKU.ap)],
                     [LA.res, MASKU.res], [pb.res], signal=(c == 3))
            return pb

        for h in range(4):
            pb = cumT(h)
            eb = EBT[h % 2]
            act(eb.ap, pb.ap, AF.Exp, [pb.res], [eb.res], scale=-1.0 / 16)
            S.op("dve", [eb.res], [EBL.res], lambda e: e.tensor_copy(EBL.ap[:, h:16:4], eb.ap[:, 127:512:128]))
            pq = psum()
            hs = slice(h * 128, (h + 1) * 128)
            S.mm(pq.ap, [(wq0[:, k, hs], HBS.ap[:, k, :]) for k in range(8)] + [(wq1[:, k, hs], HBS.ap[:, 8 + k, :]) for k in range(8)],
                 [wq_res0, wq_res1, HBS.res], [pq.res])
            S.op("dve", [pq.res, eb.res], [QT.res],
                 lambda e: e.scalar_tensor_tensor(QT.ap[:, h, :], pq.ap, 128 ** -0.5, eb.ap, ALU.mult, ALU.mult))
        wk_res0, (wk0,) = wload([wsrc(win_d, 0, 8, C_K, 512)])
        wk_res1, (wk1,) = wload([wsrc(win_d, 1024, 8, C_K, 512)])
        for h in range(4):
            pb = cumT(h)
            enb = ENBT[h % 2]
            act(enb.ap, pb.ap, AF.Exp, [pb.res], [enb.res], scale=1.0 / 16)
            pk = psum()
            hs = slice(h * 128, (h + 1) * 128)
            S.mm(pk.ap, [(wk0[:, k, hs], HBS.ap[:, k, :]) for k in range(8)] + [(wk1[:, k, hs], HBS.ap[:, 8 + k, :]) for k in range(8)],
                 [wk_res0, wk_res1, HBS.res], [pk.res])
            S.op("dve", [pk.res, enb.res], [KT.res], lambda e: e.tensor_tensor(KT.ap[:, h, :], pk.ap, enb.ap, ALU.mult))
        S.phase('gla_ktm')
        for c in range(4):
            pbt = psum()
            S.mm(pbt.ap, [(MASKU.ap, LA.ap[:, c, :])], [LA.res, MASKU.res], [pbt.res])
            act(ENBTM.ap, pbt.ap, AF.Exp, [pbt.res], [ENBTM.res], scale=1.0 / 16)
            pk = psum()
            csl_ = slice(c * 128, (c + 1) * 128)
            S.mm(pk.ap, [(HBS.ap[:, k, csl_], wk0[:, k, :]) for k in range(8)] + [(HBS.ap[:, 8 + k, csl_], wk1[:, k, :]) for k in range(8)],
                 [wk_res0, wk_res1, HBS.res], [pk.res])
            S.op("dve", [pk.res, ENBTM.res], [KTM.res], lambda e: e.tensor_tensor(KTM.ap[:, c, :], pk.ap, ENBTM.ap, ALU.mult))
        S.phase('gla_r')

        def r_evac(ch, p):
            rt = RTMP[ch % 2]
            act(rt.ap, p.ap, AF.Silu, [p.res], [rt.res])
            S.op("dve", [rt.res, GON.res], [YGLA.res],
                 lambda e: e.tensor_scalar(YGLA.ap[:, ch, :], rt.ap, GON.ap[:, ch:ch + 1], None, ALU.mult))

        fm_proj(C_R, 8, r_evac)
        S.phase('gla_loop')
        su_gen = fm_proj_gen(C_SU, 8, lambda ch, p: act(YSG.ap[:, ch, :], p.ap, AF.Gelu, [p.res], [YSG.res]))

        def scores(c):
            csl = slice(c * 128, (c + 1) * 128)
            pa = psum()
            for h in range(4):
                S.mm(pa.ap[:, h * 128:(h + 1) * 128], [(KT.ap[:, h, csl], QT.ap[:, h, csl])], [KT.res, QT.res], [pa.res],
                     signal=(h == 3))
            pt = PT4[c % 2]
            S.op("dve", [pa.res, MASK4.res], [pt.res],
                 lambda e: e.tensor_tensor(pt.ap, pa.ap.rearrange("p (h t) -> p h t", t=128), MASK4.ap, ALU.mult))

        scores(0)
        for c in range(4):
            csl = slice(c * 128, (c + 1) * 128)
            pt = PT4[c % 2]
            pB = [psum(), psum()]
            for j in range(2):
                for h in range(4):
                    S.mm(pB[j].ap[:, h * 128:(h + 1) * 128],
                         [(VTM.ap[:, c, h * 256 + j * 128:h * 256 + (j + 1) * 128], pt.ap[:, h, :]),
                          (STB4.ap[:, h, j * 128:(j + 1) * 128], QT.ap[:, h, csl])],
                         [VTM.res, pt.res, STB4.res, QT.res], [pB[j].res], signal=(h == 3))
            pKV = [psum(), psum()]
            for h in range(4):
                S.mm(pKV[h // 2].ap[:, (h % 2) * 256:(h % 2 + 1) * 256],
                     [(KTM.ap[:, c, h * 128:(h + 1) * 128], VTM.ap[:, c, h * 256:(h + 1) * 256])],
                     [KTM.res, VTM.res], [pKV[h // 2].res], signal=(h % 2 == 1))
            if c < 3:
                scores(c + 1)
            for _ in range(2):
                next(su_gen, None)
            for b in range(2):
                sv_ = ST4.ap[:, 2 * b:2 * b + 2, :]
                S.op("dve", [pKV[b].res, ST4.res], [ST4.res],
                     lambda e: e.tensor_tensor(sv_, pKV[b].ap.rearrange("p (h e) -> p h e", e=256), sv_, ALU.add))
            eblb = EBL.ap[:, c * 4:(c + 1) * 4].unsqueeze(2).to_broadcast([128, 4, 256])
            S.op("dve", [ST4.res, EBL.res], [ST4.res], lambda e: e.tensor_tensor(ST4.ap, ST4.ap, eblb, ALU.mult))
            act(STB4.ap, ST4.ap, AF.Copy, [ST4.res], [STB4.res])
            for j in range(2):
                act(OSQ8.ap[:, j * 4:(j + 1) * 4, :], pB[j].ap.rearrange("p (h t) -> p h t", t=128), AF.Square,
                    [pB[j].res], [OSQ8.res])
            pss = psum()
            for h in range(4):
                S.mm(pss.ap[:, h * 128:(h + 1) * 128], [(ONESB.ap, OSQ8.ap[:, h, :]), (ONESB.ap, OSQ8.ap[:, 4 + h, :])],
                     [ONESB.res, OSQ8.res], [pss.res], signal=(h == 3))
            rsqrt_from_psum(RS4.ap, pss.ap.rearrange("p (h t) -> p h t", t=128), 1.0 / 256, pss.res, RS4.res)
            for j in range(2):
                S.op("dve", [pB[j].res, RS4.res], [OTT.res],
                     lambda e: e.tensor_tensor(OTT.ap[:, j * 4:(j + 1) * 4, :], pB[j].ap.rearrange("p (h t) -> p h t", t=128),
                                               RS4.ap, ALU.mult))
                yv = YGLA.ap[:, j:8:2, csl]
                S.op("dve", [OTT.res, YGLA.res], [YGLA.res],
                     lambda e: e.tensor_tensor(yv, OTT.ap[:, j * 4:(j + 1) * 4, :], yv, ALU.mult))
        S.phase('sg_u')
        for _ in su_gen:
            pass
        S.phase('sg_v')
        xq_gen = fm_proj_gen(C_XQ, 8, lambda ch, p: act(XQT.ap[:, ch, :], p.ap, AF.Copy, [p.res], [XQT.res], scale=1.0 / 16))
        for cb2 in range(2):
            lsl = slice(cb2 * 512, (cb2 + 1) * 512)
            tm_proj(C_SV + cb2 * 512, lambda tt, p: act(GV4.ap[:, tt, :], p.ap, AF.Gelu, [p.res], [GV4.res]))
            for _ in range(4):
                next(xq_gen, None)
            for tt in range(4):
                for gi in range(2):
                    S.op("dve", [GV4.res], [BNS8.res],
                         lambda e: e.bn_stats(BNS8.ap[:, tt * 2 + gi, :], GV4.ap[:, tt, gi * 256:(gi + 1) * 256]))
            for q8 in range(8):
                S.op("dve", [BNS8.res], [BNA8.res], lambda e: e.bn_aggr(BNA8.ap[:, q8, :], BNS8.ap[:, q8, :]))
            act(LNR8.ap, BNA8.ap[:, :, 1], AF.Ln, [BNA8.res], [LNR8.res], bias=EPS)
            act(LNR8.ap, LNR8.ap, AF.Exp, [LNR8.res], [LNR8.res], scale=-0.5)
            for tt in range(4):
                for gi in range(2):
                    gsl = slice(gi * 256, (gi + 1) * 256)
                    q8 = tt * 2 + gi
                    S.op("dve", [GV4.res, BNA8.res, LNR8.res], [GV4.res],
                         lambda e: e.tensor_scalar(GV4.ap[:, tt, gsl], GV4.ap[:, tt, gsl], BNA8.ap[:, q8, 0:1], LNR8.ap[:, q8:q8 + 1],
                                                   ALU.subtract, ALU.mult))
            lg = LNBC.ap[:, lsl].unsqueeze(1).to_broadcast([128, 4, 512])
            lb = LNBC.ap[:, 1024 + cb2 * 512:1024 + (cb2 + 1) * 512].unsqueeze(1).to_broadcast([128, 4, 512])
            S.op("dve", [GV4.res, LNBC.res], [GV4.res], lambda e: e.tensor_tensor(GV4.ap, GV4.ap, lg, ALU.mult))
            S.op("dve", [GV4.res, LNBC.res], [SVTM.res], lambda e: e.tensor_tensor(SVTM.ap[:, :, lsl], GV4.ap, lb, ALU.add))
        S.phase('sg_mix')
        for g in range(4):
            for cj in range(2):
                p = psum()
                for tt in range(4):
                    S.mm(p.ap[:, tt * 128:(tt + 1) * 128],
                         [(SVTM.ap[:, tt, g * 256 + cj * 128:g * 256 + (cj + 1) * 128], WST.ap[:, g, :]),
                          (ONESF.ap[0:1, :], BSROW.ap[0:1, g * 128:(g + 1) * 128])],
                         [SVTM.res, WST.res, ONESF.res, BSROW.res], [p.res], signal=(tt == 3))
                yv = YSG.ap[:, g * 2 + cj, :]
                S.op("dve", [p.res, YSG.res], [YSG.res], lambda e: e.tensor_tensor(yv, p.ap, yv, ALU.mult))
        S.phase('xa_q')
        for _ in xq_gen:
            pass
        S.phase('xa_attn')
        def xa_scores(tt):
            tsl_ = slice(tt * 128, (tt + 1) * 128)
            psc_ = [psum(), psum()]
            for h in range(4):
                S.mm(psc_[h // 2].ap[:, (h % 2) * 256:(h % 2 + 1) * 256],
                     [(XQT.ap[:, h * 2 + j, tsl_], MKT.ap[:, h * 2 + j, :]) for j in range(2)],
                     [XQT.res, MKT.res], [psc_[h // 2].res], signal=(h % 2 == 1))
            return psc_

        psc_next = xa_scores(0)
        for tt in range(4):
            tsl = slice(tt * 128, (tt + 1) * 128)
            pf, pt2 = PF[tt % 2], PT2[tt % 2]
            psc = psc_next
            if tt < 3:
                psc_next = xa_scores(tt + 1)
            for b in range(2):
                S.op("dve", [psc[b].res], [MXB.res],
                     lambda e: e.tensor_reduce(MXB.ap[:, 2 * b:2 * b + 2], psc[b].ap.rearrange("p (h m) -> p h m", m=256), AX.X, ALU.max))
            S.op("dve", [MXB.res], [NMX.res], lambda e: e.tensor_scalar(NMX.ap, MXB.ap, -1.0, None, ALU.mult))
            for h in range(4):
                act(pf.ap[:, h, :], psc[h // 2].ap[:, (h % 2) * 256:(h % 2 + 1) * 256], AF.Exp,
                    [psc[h // 2].res, NMX.res], [pf.res, RSUM.res], bias=NMX.ap[:, h:h + 1], accum_out=RSUM.ap[:, h:h + 1])
            S.op("dve", [RSUM.res], [RINV.res], lambda e: e.reciprocal(RINV.ap, RSUM.ap))
            for h in range(4):
                S.op("dve", [pf.res, RINV.res], [pf.res],
                     lambda e: e.tensor_scalar(pf.ap[:, h, :], pf.ap[:, h, :], RINV.ap[:, h:h + 1], None, ALU.mult))
            for b in range(2):
                ptp = psum()
                for q in range(4):
                    h, mt = b * 2 + q // 2, q % 2
                    S.tr(ptp.ap[:, q * 128:(q + 1) * 128], pf.ap[:, h, mt * 128:(mt + 1) * 128], IDENT.ap,
                         [pf.res, IDENT.res], [ptp.res], signal=(q == 3))
                ov = pt2.ap[:, b * 4:(b + 1) * 4, :]
                iv = ptp.ap.rearrange("p (a b) -> p a b", b=128)
                if b == 0:
                    act(ov, iv, AF.Copy, [ptp.res], [pt2.res])
                else:
                    S.op("dve", [ptp.res], [pt2.res], lambda e: e.tensor_copy(ov, iv))
            for b in range(2):
                po = psum()
                for q in range(4):
                    hj = b * 4 + q
                    h, j = hj // 2, hj % 2
                    S.mm(po.ap[:, q * 128:(q + 1) * 128],
                         [(MV.ap[:, mt, h * 256 + j * 128:h * 256 + (j + 1) * 128], pt2.ap[:, h * 2 + mt, :]) for mt in range(2)],
                         [MV.res, pt2.res], [po.res], signal=(q == 3))
                ov = YXA.ap[:, b * 4:(b + 1) * 4, tsl]
                iv = po.ap.rearrange("p (a b) -> p a b", b=128)
                if b == 0:
                    act(ov, iv, AF.Copy, [po.res], [YXA.res])
                else:
                    S.op("dve", [po.res], [YXA.res], lambda e: e.tensor_copy(ov, iv))
        ys = [YGLA, YSG, YXA]
        S.phase('merge')
        for jg in range(4):
            mrg = MRG[jg % 2]
            for i in range(3):
                gt = GT[i % 2]
                gbanks = [psum() for _ in range(4)]
                for kh in range(2):
                    wres, (wv,) = wload([wsrc(win_d, kh * 1024, 8, C_GATE + i * 2048 + jg * 512, 512)])
                    for ji in range(4):
                        p = gbanks[ji]
                        S.mm(p.ap, [(wv[:, k, ji * 128:(ji + 1) * 128], HBS.ap[:, kh * 8 + k, :]) for k in range(8)],
                             [wres, HBS.res], [p.res], start=(kh == 0), stop=(kh == 1))
                        if kh == 1:
                            act(gt.ap[:, ji, :], p.ap, AF.Sigmoid, [p.res], [gt.res])
                wres, (wv,) = wload([wsrc(wbr_d, i * 1024, 8, jg * 512, 512)])
                for ji in range(4):
                    p = psum()
                    S.mm(p.ap, [(wv[:, k, ji * 128:(ji + 1) * 128], ys[i].ap[:, k, :]) for k in range(8)], [wres, ys[i].res], [p.res])
                    if i == 0:
                        S.op("dve", [p.res, gt.res], [ACC.res], lambda e: e.tensor_tensor(ACC.ap[:, ji, :], gt.ap[:, ji, :], p.ap, ALU.mult))
                    else:
                        tt_ = TT_[ji % 2]
                        S.op("dve", [p.res, gt.res], [tt_.res], lambda e: e.tensor_tensor(tt_.ap, gt.ap[:, ji, :], p.ap, ALU.mult))
                        if i == 1:
                            S.op("dve", [tt_.res, ACC.res], [ACC.res], lambda e: e.tensor_tensor(ACC.ap[:, ji, :], ACC.ap[:, ji, :], tt_.ap, ALU.add))
                        else:
                            S.op("dve", [tt_.res, ACC.res], [mrg.res], lambda e: e.tensor_tensor(mrg.ap[:, ji, :], ACC.ap[:, ji, :], tt_.ap, ALU.add))
            for oh in range(2):
                wres, (wv,) = wload([wsrc(wout_d, jg * 512, 4, oh * 1024, 1024)])
                for d8 in range(8):
                    dc = oh * 8 + d8
                    p = psum()
                    S.mm(p.ap, [(wv[:, k, d8 * 128:(d8 + 1) * 128], mrg.ap[:, k, :]) for k in range(4)], [wres, mrg.res], [p.res])
                    xv = X.ap[:, dc, t0:t0 + 512]
                    S.op("dve", [p.res, X.res], [X.res], lambda e: e.tensor_tensor(xv, p.ap, xv, ALU.add))

    for tile in range(2):
        row0 = tile * 1024
        S.phase('load_x')
        load_fm(lambda tt: x_d[row0 + tt * 128: row0 + (tt + 1) * 128, :], X, 8, STG)
        ffn(*ffw[0], 0)
        for sub in range(2):
            mixer(sub * 512)
        ffn(*ffw[1], 32)
        S.phase('final_norm')
        rmsnorm(X, 0, X, 0, 48, 1024, E_OFF)
        S.phase('store')
        store_fm(X, row0, 8, STG)
    for k in ("out0", "out1"):
        nc.sync.wait_ge(S.sem[k], S.cnt[k])
    S.phase('end')
    nc._phases = S.phases
    return nc


_NC_CACHE = {}


def _fm(v):
    return np.ascontiguousarray(np.asarray(v, np.float32).reshape(16, 128).T)


def kernel(x, mem, ffn1_norm, ffn1_w_gate, ffn1_w_up, ffn1_w_down, mix_norm, mem_norm,
           w_in, gla_w_gate_up, gla_gate_bias, gla_out_norm, sg_ln_g, sg_ln_b, sg_w_s, sg_b_s,
           w_kv_mem, w_branch, w_out, ffn2_norm, ffn2_w_gate, ffn2_w_up, ffn2_w_down, final_norm):
    f = lambda a: np.ascontiguousarray(np.asarray(a, np.float32))
    x = f(x)
    mem = f(mem)
    B = x.shape[0]
    gains = np.concatenate([_fm(f(ffn1_norm)[0]), _fm(f(mix_norm)[0]), _fm(f(ffn2_norm)[0]),
                            _fm(f(final_norm)), _fm(f(mem_norm)[0])], axis=1)
    gon = np.ascontiguousarray(f(gla_out_norm)[0].reshape(8, 128).T)
    lnbc = np.ascontiguousarray(np.broadcast_to(
        np.concatenate([f(sg_ln_g)[0].reshape(-1), f(sg_ln_b)[0].reshape(-1)])[None, :], (128, 2048)))
    wst = np.ascontiguousarray(f(sg_w_s)[0].transpose(2, 0, 1).reshape(128, 512))
    wgu = np.ascontiguousarray(np.concatenate([f(gla_w_gate_up)[0], f(gla_gate_bias)[0][None, :]], axis=0))
    bs = np.ascontiguousarray(f(sg_b_s)[0].reshape(1, 512))
    cst = np.ascontiguousarray(np.concatenate([np.eye(128, dtype=np.float32),
                                               np.triu(np.ones((128, 128), np.float32))], axis=1))
    shared = {
        "w_in": f(w_in)[0], "f1g": f(ffn1_w_gate)[0], "f1u": f(ffn1_w_up)[0], "f1d": f(ffn1_w_down)[0],
        "f2g": f(ffn2_w_gate)[0], "f2u": f(ffn2_w_up)[0], "f2d": f(ffn2_w_down)[0],
        "w_kv": f(w_kv_mem)[0], "w_br": f(w_branch)[0].reshape(3072, 2048), "w_out": f(w_out)[0],
        "gains": gains, "gon": gon, "lnbc": lnbc, "wst": wst, "wgu": wgu, "bs": bs, "cst": cst,
    }
    if "nc" not in _NC_CACHE:
        _NC_CACHE["nc"] = build_nc()
    nc = _NC_CACHE["nc"]
    in_maps = []
    for b in range(B):
        m = dict(shared)
        m["x"] = x[b]
        m["mem"] = mem[b]
        in_maps.append(m)
    res = run_bass_kernel_spmd(nc, in_maps, core_ids=list(range(B)))
    return np.stack([np.asarray(r["y"], np.float32) for r in res.results], axis=0)
```
